# Optimizing a Trainium2 kernel written in Bass

```python
import jax, jax.numpy as jnp
from jax import lax
import numpy as np


D_MODEL = 4096
BATCH = 1
SEQ = 16384
DEPTH = 2

N_GROUPS = 4
GROUP_WIDTH = D_MODEL // N_GROUPS
MIX_WIDTH = N_GROUPS * GROUP_WIDTH
CHUNK = 64
EPS = 1e-6
GLA_HEADS = 4
GLA_DV = GROUP_WIDTH // GLA_HEADS
GLA_DK = GLA_DV // 2
GLA_LOWRANK = 16
GLA_GATE_NORMALIZER = 16.0
RG_BLOCKS = 8
RG_BLOCK = GROUP_WIDTH // RG_BLOCKS
RG_CONV = 4
RG_C = 8.0
RG_A_MIN = 0.9
RG_A_MAX = 0.999
ML_HEADS = 4
ML_DV = GROUP_WIDTH // ML_HEADS
ML_DK = ML_DV // 2
ML_FGATE_BIAS = 3.0
ML_IGATE_BIAS = -2.0
HG_HEADS = 4
HG_DV = GROUP_WIDTH // HG_HEADS
HG_DK = HG_DV // 2
D_FF = 11008
N_EXPERTS = 8
TOP_K = 2
D_FF_EXPERT = D_FF // 2
N_DENSE = (DEPTH + 1) // 2
N_MOE = DEPTH // 2
PLE_DIM = 256

SPLIT_SIZES = (
    GLA_HEADS * GLA_DK, GLA_HEADS * GLA_DK, GROUP_WIDTH, GROUP_WIDTH, GLA_LOWRANK,
    GROUP_WIDTH, GROUP_WIDTH,
    ML_HEADS * ML_DK, ML_HEADS * ML_DK, GROUP_WIDTH, GROUP_WIDTH, ML_HEADS, ML_HEADS,
    HG_HEADS * HG_DK, HG_HEADS * HG_DK, GROUP_WIDTH, GROUP_WIDTH,
)
PROJ_WIDTH = sum(SPLIT_SIZES)

kernel_name = 'hymba_style_gla_rglru_mlstm_hgrn2_moe'


def rmsnorm(x, gain):
    xf = x.astype(jnp.float32)
    y = xf * lax.rsqrt(jnp.mean(xf * xf, axis=-1, keepdims=True) + EPS)
    return (y * gain.astype(jnp.float32)).astype(x.dtype)


def heads(t, n):
    return t.reshape(t.shape[:-1] + (n, t.shape[-1] // n))


def to_chunks(t):
    b, s = t.shape[:2]
    t = t.reshape((b, s // CHUNK, CHUNK) + t.shape[2:])
    return jnp.swapaxes(jnp.moveaxis(t, 1, 0), 2, 3)


def from_chunks(t):
    t = jnp.moveaxis(jnp.swapaxes(t, 2, 3), 0, 1)
    return t.reshape((t.shape[0], t.shape[1] * t.shape[2]) + t.shape[3:])


def chunk_gla(q, k, v, log_a):
    f32 = jnp.float32
    q, k, v, log_a = (t.astype(f32) for t in (q, k, v, log_a))
    bsz, _, nh, dk = q.shape
    dv = v.shape[-1]
    causal = jnp.tril(jnp.ones((CHUNK, CHUNK), bool))

    def step(state, inp):
        qi, ki, vi, ai = inp
        b = jnp.cumsum(ai, axis=-2)
        diff = b[..., :, None, :] - b[..., None, :, :]
        decay = jnp.exp(jnp.where(causal[:, :, None], diff, -jnp.inf))
        scores = jnp.einsum('bhtd,bhsd,bhtsd->bhts', qi, ki, decay)
        o = (jnp.einsum('bhts,bhsv->bhtv', scores, vi)
             + jnp.einsum('bhtd,bhdv->bhtv', qi * jnp.exp(b), state))
        b_last = b[..., -1:, :]
        k_dec = ki * jnp.exp(b_last - b)
        state = (state * jnp.exp(b[..., -1, :])[..., None]
                 + jnp.einsum('bhsd,bhsv->bhdv', k_dec, vi))
        return state, o

    s0 = jnp.zeros((bsz, nh, dk, dv), f32)
    _, o = lax.scan(step, s0, tuple(to_chunks(t) for t in (q, k, v, log_a)))
    return from_chunks(o)


def chunk_mlstm(q, k, v, i_pre, log_f):
    f32 = jnp.float32
    q, k, v, i_pre, log_f = (t.astype(f32) for t in (q, k, v, i_pre, log_f))
    bsz, _, nh, dk = q.shape
    dv = v.shape[-1]
    causal = jnp.tril(jnp.ones((CHUNK, CHUNK), bool))

    def step(carry, inp):
        c_st, n_st, m_st = carry
        qi, ki, vi, ii, fi = inp
        b = jnp.cumsum(fi, axis=-1)
        dlog = jnp.where(causal, b[..., :, None] - b[..., None, :] + ii[..., None, :], -jnp.inf)
        g = b + m_st[..., None]
        m_t = jnp.maximum(g, jnp.max(dlog, axis=-1))
        w = jnp.exp(dlog - m_t[..., None])
        inter = jnp.exp(g - m_t)
        qk = jnp.einsum('bhtd,bhsd->bhts', qi, ki) * w
        num = (jnp.einsum('bhts,bhsv->bhtv', qk, vi)
               + inter[..., None] * jnp.einsum('bhtd,bhdv->bhtv', qi, c_st))
        den = jnp.sum(qk, axis=-1) + inter * jnp.einsum('bhtd,bhd->bht', qi, n_st)
        h = num / jnp.maximum(jnp.abs(den), jnp.exp(-m_t))[..., None]
        b_last = b[..., -1]
        g_last = b_last + m_st
        d_last = b_last[..., None] - b + ii
        m_new = jnp.maximum(g_last, jnp.max(d_last, axis=-1))
        ws = jnp.exp(d_last - m_new[..., None])
        sc = jnp.exp(g_last - m_new)
        c_new = sc[..., None, None] * c_st + jnp.einsum('bhs,bhsd,bhsv->bhdv', ws, ki, vi)
        n_new = sc[..., None] * n_st + jnp.einsum('bhs,bhsd->bhd', ws, ki)
        return (c_new, n_new, m_new), h

    init = (jnp.zeros((bsz, nh, dk, dv), f32), jnp.zeros((bsz, nh, dk), f32), jnp.zeros((bsz, nh), f32))
    _, h = lax.scan(step, init, tuple(to_chunks(t) for t in (q, k, v, i_pre, log_f)))
    return from_chunks(h)


def gla_group(q, k, v, g, lr, w_up, b_up, norm_gain):
    f32 = jnp.float32
    log_a = jax.nn.log_sigmoid((lr @ w_up + b_up).astype(f32)) / GLA_GATE_NORMALIZER
    o = chunk_gla(heads(q, GLA_HEADS) * (GLA_DK ** -0.5), heads(k, GLA_HEADS),
                  heads(v, GLA_HEADS), heads(log_a, GLA_HEADS))
    o = rmsnorm(o, norm_gain) * jax.nn.silu(heads(g, GLA_HEADS).astype(f32))
    return o.reshape(o.shape[:2] + (GROUP_WIDTH,))


def rglru_group(xb, gate, conv_w, conv_b, w_a, b_a, w_x, b_x, lam):
    f32 = jnp.float32
    xc = lax.conv_general_dilated(xb, conv_w[:, None, :].astype(xb.dtype), window_strides=(1,),
                                  padding=[(RG_CONV - 1, 0)], dimension_numbers=('NWC', 'WIO', 'NWC'),
                                  feature_group_count=GROUP_WIDTH) + conv_b
    xh = heads(xc, RG_BLOCKS)
    r = jax.nn.sigmoid((jnp.einsum('bsnc,ncd->bsnd', xh, w_a).reshape(xc.shape) + b_a).astype(f32))
    i = jax.nn.sigmoid((jnp.einsum('bsnc,ncd->bsnd', xh, w_x).reshape(xc.shape) + b_x).astype(f32))
    log_a = -RG_C * r * jax.nn.softplus(-lam.astype(f32))
    a = jnp.exp(log_a)
    u = jnp.sqrt(-jnp.expm1(2.0 * log_a)) * (i * xc.astype(f32))

    def combine(c1, c2):
        a1, b1 = c1
        a2, b2 = c2
        return a1 * a2, a2 * b1 + b2

    _, h = lax.associative_scan(combine, (a, u), axis=1)
    return jax.nn.gelu(gate.astype(f32)) * h


def mlstm_group(q, k, v, o, i_pre, f_pre, b_i, b_f, norm_gain):
    f32 = jnp.float32
    i_t = (i_pre + b_i).astype(f32)
    log_f = jax.nn.log_sigmoid((f_pre + b_f).astype(f32))
    h = chunk_mlstm(heads(q, ML_HEADS) * (ML_DK ** -0.5), heads(k, ML_HEADS), heads(v, ML_HEADS), i_t, log_f)
    h = rmsnorm(h, heads(norm_gain, ML_HEADS)) * jax.nn.sigmoid(heads(o, ML_HEADS).astype(f32))
    return h.reshape(h.shape[:2] + (GROUP_WIDTH,))


def hgrn2_group(q, f_pre, i, g, lb, norm_gain):
    f32 = jnp.float32
    fp = f_pre.astype(f32)
    log_f = jnp.logaddexp(jnp.log(lb), jnp.log1p(-lb) + jax.nn.log_sigmoid(fp))
    k = (1.0 - lb) * jax.nn.sigmoid(-fp)
    o = chunk_gla(heads(jax.nn.silu(q.astype(f32)), HG_HEADS) * (HG_DK ** -0.5), heads(k, HG_HEADS),
                  heads(i, HG_HEADS), heads(log_f, HG_HEADS))
    o = rmsnorm(o, norm_gain) * jax.nn.silu(heads(g, HG_HEADS).astype(f32))
    return o.reshape(o.shape[:2] + (GROUP_WIDTH,))


def swiglu(x, w1, w3, w2):
    return (jax.nn.silu(x @ w1) * (x @ w3)) @ w2


def moe_top2(x, router, w1, w3, w2):
    f32 = jnp.float32
    logits = (x @ router).astype(f32)
    vals, idx = lax.top_k(logits, TOP_K)
    gates = jax.nn.softmax(vals, axis=-1)
    combine = jnp.sum(jax.nn.one_hot(idx, N_EXPERTS, dtype=f32) * gates[..., None], axis=-2)
    out = jnp.zeros(x.shape, f32)
    for e in range(N_EXPERTS):
        out = out + combine[..., e:e + 1] * swiglu(x, w1[e], w3[e], w2[e]).astype(f32)
    return out.astype(x.dtype)


def _normal(key, shape, scale):
    return jax.random.normal(key, shape, jnp.float32) * scale


def setup_inputs(seed: int = 0) -> dict:
    key = jax.random.key(seed)
    ks = jax.random.split(key, 32)
    G = GROUP_WIDTH
    u = jax.random.uniform(ks[12], (DEPTH, G), jnp.float32, RG_A_MIN, RG_A_MAX)
    s = u ** (1.0 / RG_C)
    rg_lambda = jnp.log(s) - jnp.log1p(-s)
    return {
        'x': _normal(ks[0], (BATCH, SEQ, D_MODEL), 1.0),
        'p': _normal(ks[1], (DEPTH, BATCH, SEQ, PLE_DIM), 1.0),
        'attn_norm': 1.0 + _normal(ks[2], (DEPTH, D_MODEL), 0.05),
        'w_in': _normal(ks[3], (DEPTH, D_MODEL, PROJ_WIDTH), D_MODEL ** -0.5),
        'w_out': _normal(ks[4], (DEPTH, MIX_WIDTH, D_MODEL), MIX_WIDTH ** -0.5),
        'gla_w_up': _normal(ks[5], (DEPTH, GLA_LOWRANK, GLA_HEADS * GLA_DK), GLA_LOWRANK ** -0.5),
        'gla_b_up': _normal(ks[6], (DEPTH, GLA_HEADS * GLA_DK), 0.1),
        'gla_norm': 1.0 + _normal(ks[7], (DEPTH, GLA_DV), 0.05),
        'rg_conv_w': _normal(ks[8], (DEPTH, RG_CONV, G), RG_CONV ** -0.5),
        'rg_conv_b': _normal(ks[9], (DEPTH, G), 0.02),
        'rg_w_a': _normal(ks[10], (DEPTH, RG_BLOCKS, RG_BLOCK, RG_BLOCK), RG_BLOCK ** -0.5),
        'rg_b_a': _normal(ks[11], (DEPTH, G), 0.1),
        'rg_w_x': _normal(ks[13], (DEPTH, RG_BLOCKS, RG_BLOCK, RG_BLOCK), RG_BLOCK ** -0.5),
        'rg_b_x': _normal(ks[14], (DEPTH, G), 0.1),
        'rg_lambda': rg_lambda,
        'ml_b_i': ML_IGATE_BIAS + _normal(ks[15], (DEPTH, ML_HEADS), 0.1),
        'ml_b_f': ML_FGATE_BIAS + _normal(ks[16], (DEPTH, ML_HEADS), 0.1),
        'ml_norm': 1.0 + _normal(ks[17], (DEPTH, G), 0.05),
        'hg_lb_logits': _normal(ks[18], (DEPTH, HG_HEADS * HG_DK), 1.0),
        'hg_norm': 1.0 + _normal(ks[19], (DEPTH, HG_DV), 0.05),
        'ffn_norm': 1.0 + _normal(ks[20], (DEPTH, D_MODEL), 0.05),
        'ffn_w1': _normal(ks[21], (N_DENSE, D_MODEL, D_FF), D_MODEL ** -0.5),
        'ffn_w3': _normal(ks[22], (N_DENSE, D_MODEL, D_FF), D_MODEL ** -0.5),
        'ffn_w2': _normal(ks[23], (N_DENSE, D_FF, D_MODEL), D_FF ** -0.5),
        'moe_router': _normal(ks[24], (N_MOE, D_MODEL, N_EXPERTS), D_MODEL ** -0.5),
        'moe_w1': _normal(ks[25], (N_MOE, N_EXPERTS, D_MODEL, D_FF_EXPERT), D_MODEL ** -0.5),
        'moe_w3': _normal(ks[26], (N_MOE, N_EXPERTS, D_MODEL, D_FF_EXPERT), D_MODEL ** -0.5),
        'moe_w2': _normal(ks[27], (N_MOE, N_EXPERTS, D_FF_EXPERT, D_MODEL), D_FF_EXPERT ** -0.5),
        'ple_norm': 1.0 + _normal(ks[28], (DEPTH, D_MODEL), 0.05),
        'ple_w_gate': _normal(ks[29], (DEPTH, D_MODEL, D_MODEL), D_MODEL ** -0.5),
        'ple_w_proj': _normal(ks[30], (DEPTH, PLE_DIM, D_MODEL), PLE_DIM ** -0.5),
        'final_norm': 1.0 + _normal(ks[31], (D_MODEL,), 0.05),
    }


def reference(x, p, attn_norm, w_in, w_out, gla_w_up, gla_b_up, gla_norm, rg_conv_w, rg_conv_b,
              rg_w_a, rg_b_a, rg_w_x, rg_b_x, rg_lambda, ml_b_i, ml_b_f, ml_norm, hg_lb_logits, hg_norm,
              ffn_norm, ffn_w1, ffn_w3, ffn_w2, moe_router, moe_w1, moe_w3, moe_w2,
              ple_norm, ple_w_gate, ple_w_proj, final_norm):
    split_at = [int(c) for c in np.cumsum(SPLIT_SIZES)[:-1]]
    sm = jax.nn.softmax(hg_lb_logits.astype(jnp.float32), axis=0)
    lb_all = jnp.cumsum(jnp.where(jnp.arange(DEPTH)[:, None] > 0, sm, 0.0), axis=0)
    h = x
    for l in range(DEPTH):
        hn = rmsnorm(h, attn_norm[l])
        (ga_q, ga_k, ga_v, ga_g, ga_lr, rg_x, rg_gate, ml_q, ml_k, ml_v, ml_o, ml_i, ml_f,
         hg_q, hg_f, hg_i, hg_g) = jnp.split(hn @ w_in[l], split_at, axis=-1)
        y_a = gla_group(ga_q, ga_k, ga_v, ga_g, ga_lr, gla_w_up[l], gla_b_up[l], gla_norm[l])
        y_b = rglru_group(rg_x, rg_gate, rg_conv_w[l], rg_conv_b[l], rg_w_a[l], rg_b_a[l],
                          rg_w_x[l], rg_b_x[l], rg_lambda[l])
        y_c = mlstm_group(ml_q, ml_k, ml_v, ml_o, ml_i, ml_f, ml_b_i[l], ml_b_f[l], ml_norm[l])
        y_d = hgrn2_group(hg_q, hg_f, hg_i, hg_g, lb_all[l], hg_norm[l])
        mixed = jnp.concatenate([y_a, y_b, y_c, y_d], axis=-1).astype(h.dtype)
        h = h + mixed @ w_out[l]
        hn = rmsnorm(h, ffn_norm[l])
        j = l // 2
        if l % 2 == 0:
            h = h + swiglu(hn, ffn_w1[j], ffn_w3[j], ffn_w2[j])
        else:
            h = h + moe_top2(hn, moe_router[j], moe_w1[j], moe_w3[j], moe_w2[j])
        gate = jax.nn.sigmoid((rmsnorm(h, ple_norm[l]) @ ple_w_gate[l]).astype(jnp.float32))
        h = h + (gate * (p[l] @ ple_w_proj[l]).astype(jnp.float32)).astype(h.dtype)
    return rmsnorm(h, final_norm)
```

```python
from contextlib import ExitStack
import numpy as np
import concourse.bass as bass
import concourse.mybir as mybir
from concourse.bass_utils import run_bass_kernel_spmd

F32 = mybir.dt.float32
BF16 = mybir.dt.bfloat16
AF = mybir.ActivationFunctionType
ALU = mybir.AluOpType
EPS = 1e-6
SEM_MAX = 30000


class Reg:
    __slots__ = ("w", "r", "wpe", "ctr")

    def __init__(self):
        self.w = None
        self.r = {}
        self.wpe = False
        self.ctr = None


class Ctr:
    def __init__(self, ctx):
        self.c = ctx
        self.sems = []
        self.n = 0
        self.base = 0
        self.finals = []

    def _roll(self, inc):
        if not self.sems or self.n - self.base + inc > SEM_MAX:
            if self.sems:
                self.finals.append((self.sems[-1], self.n - self.base))
            self.sems.append(self.c.sem())
            self.base = self.n

    def bump(self, inc):
        self._roll(inc)
        self.n += inc
        return (self.sems[-1], self.n - self.base)

    def peek(self, inc):
        self._roll(inc)
        return (self.sems[-1], self.n - self.base + inc)


class Ctx:
    def __init__(self, nc):
        self.nc = nc
        self.es = ExitStack()
        self.eng = dict(pe=nc.tensor, act=nc.scalar, dve=nc.vector, pool=nc.gpsimd, sp=nc.sync)
        self.nsem = 0
        self.pc = {e: Ctr(self) for e in self.eng}
        self.known = {e: {} for e in self.eng}
        self.dctrs = []

    def sem(self):
        self.nsem += 1
        h = self.es.enter_context(self.nc.semaphore(f"s{self.nsem}"))
        return (h, self.nsem)

    def sb(self, name, shape, dt=F32):
        return self.es.enter_context(self.nc.sbuf_tensor("sb_" + name, shape, dt))

    def ps(self, name, shape, dt=F32):
        return self.es.enter_context(self.nc.psum_tensor("ps_" + name, shape, dt))

    def _waits(self, e, reads, writes):
        deps = {}

        def add(ev):
            if ev is None:
                return
            sm, v = ev
            if sm[1] not in deps or deps[sm[1]][1] < v:
                deps[sm[1]] = (sm, v)

        for r in reads:
            add(r.w)
        for w in writes:
            if not (e == "pe" and w.wpe):
                add(w.w)
            for ev in w.r.values():
                add(ev)
        kn = self.known[e]
        for sid, (sm, v) in deps.items():
            if kn.get(sid, 0) >= v:
                continue
            self.eng[e].wait_ge(sm[0], v)
            kn[sid] = v

    def op(self, e, fn, reads=(), writes=(), sig=True):
        self._waits(e, reads, writes)
        ins = fn(self.eng[e])
        if sig:
            ev = self.pc[e].bump(1)
            ins.then_inc(ev[0][0], 1)
        else:
            ev = self.pc[e].peek(1)
        for r in reads:
            r.r[ev[0][1]] = ev
        for w in writes:
            w.w = ev
            w.r = {}
            w.wpe = e == "pe"

    def dma(self, q, out, in_, sreg, reads=(), writes=()):
        self._waits(q, reads, writes)
        if sreg.ctr is None:
            sreg.ctr = Ctr(self)
            self.dctrs.append(sreg.ctr)
        ev = sreg.ctr.bump(16)
        self.eng[q].dma_start(out=out, in_=in_).then_inc(ev[0][0], 16)
        for r in reads:
            r.r[ev[0][1]] = ev
        for w in writes:
            w.w = ev
            w.r = {}
            w.wpe = False

    def finish(self):
        sp = self.eng["sp"]
        for ct in self.dctrs:
            for sm, v in ct.finals:
                sp.wait_ge(sm[0], v)
            if ct.sems:
                sp.wait_ge(ct.sems[-1][0], ct.n - ct.base)
        self.es.close()


def make_cfg(D=4096, T=2048, NCORE=8, DFF=11008, DFFE=5504, NE=8):
    return dict(D=D, KC=D // 128, T=T, NST=T // 512, NCORE=NCORE, DFF=DFF, DFFE=DFFE, NE=NE,
                NH=DFF // 128, NHE=DFFE // 128, NCT=D // 256)


O_GAQ, O_GAK, O_GAV, O_GAG, O_GALR = 0, 512, 1024, 2048, 3072
O_RGX, O_RGG = 3088, 4112
O_MLQ, O_MLK, O_MLV, O_MLO, O_MLI, O_MLF = 5136, 5648, 6160, 7184, 8208, 8212
O_HGQ, O_HGF, O_HGI, O_HGG = 8216, 8728, 9240, 10264
DKS = 128 ** -0.5

NFCH = 73


def fchunk_cols():
    cols = [list(range(O_GALR, O_GALR + 16)) + [-1] * 112]
    for h in range(4):
        cols.append(list(range(O_GAQ + 128 * h, O_GAQ + 128 * h + 128)))
        cols.append(list(range(O_GAK + 128 * h, O_GAK + 128 * h + 128)))
        cols.append(list(range(O_GAG + 256 * h, O_GAG + 256 * h + 128)))
        cols.append(list(range(O_GAG + 256 * h + 128, O_GAG + 256 * h + 256)))
    for n in range(8):
        cols.append(list(range(O_RGX + 128 * n, O_RGX + 128 * n + 128)))
        cols.append(list(range(O_RGG + 128 * n, O_RGG + 128 * n + 128)))
    for h in range(4):
        cols.append(list(range(O_MLQ + 128 * h, O_MLQ + 128 * h + 128)))
        cols.append(list(range(O_MLK + 128 * h, O_MLK + 128 * h + 128)))
        cols.append(list(range(O_MLO + 256 * h, O_MLO + 256 * h + 128)))
        cols.append(list(range(O_MLO + 256 * h + 128, O_MLO + 256 * h + 256)))
        cols.append([O_MLI + h] * 128)
        cols.append([O_MLF + h] * 128)
    for h in range(4):
        cols.append(list(range(O_HGQ + 128 * h, O_HGQ + 128 * h + 128)))
        cols.append(list(range(O_HGF + 128 * h, O_HGF + 128 * h + 128)))
        cols.append(list(range(O_HGG + 256 * h, O_HGG + 256 * h + 128)))
        cols.append(list(range(O_HGG + 256 * h + 128, O_HGG + 256 * h + 256)))
    assert len(cols) == NFCH
    return cols


V_OFF = [O_GAV + 256 * h for h in range(4)] + [O_MLV + 256 * h for h in range(4)] + [O_HGI + 256 * h for h in range(4)]


def make_ident(c, n=128):
    ident = c.sb("ident", [128, 128], F32)
    r = Reg()
    c.op("pool", lambda g: g.memset(ident[:], 1.0), writes=[r])
    c.op("pool", lambda g: g.affine_select(out=ident[:], in_=ident[:], pattern=[[-1, 128]],
                                           compare_op=ALU.is_equal, fill=0.0, base=0,
                                           channel_multiplier=1), reads=[r], writes=[r])
    return ident, r


def emit_norm_tile(c, cfg, B, src_ap, tok0, ntok_cols, xT, r_xT, gain, r_gain, rstd_out=None, keep=None, router=None):
    D, KC = cfg["D"], cfg["KC"]
    DH, KH = D // 2, KC // 2
    G = min(4, KH)
    hs, r_hs, hn, r_hn, ss, r_ss, ptr, r_ptr, ident, r_id = B["norm"]
    if keep is None:
        c.dma("sp", hs[:], src_ap, r_hs, writes=[r_hs])
        sap, r_src = hs[:], r_hs
    else:
        sap, r_src = keep
    for hf in range(2):
        c.op("act", lambda a, hf=hf: a.activation(out=hn[:], in_=sap[:, hf * DH:(hf + 1) * DH], func=AF.Square,
                                                  accum_out=ss[:, 4 + hf:5 + hf]),
             reads=[r_src], writes=[r_hn, r_ss])
    c.op("dve", lambda v: v.tensor_tensor(out=ss[:, 0:1], in0=ss[:, 4:5], in1=ss[:, 5:6], op=ALU.add),
         reads=[r_ss], writes=[r_ss])
    c.op("dve", lambda v: v.tensor_scalar(out=ss[:, 1:2], in0=ss[:, 0:1], scalar1=1.0 / D, scalar2=EPS,
                                          op0=ALU.mult, op1=ALU.add), reads=[r_ss], writes=[r_ss])
    c.op("act", lambda a: a.sqrt(out=ss[:, 3:4], in_=ss[:, 1:2]), reads=[r_ss], writes=[r_ss])
    c.op("dve", lambda v: v.reciprocal(out=ss[:, 2:3], in_=ss[:, 3:4]), reads=[r_ss], writes=[r_ss])
    gi_ = 0
    for hf in range(2):
        c.op("dve", lambda v, hf=hf: v.tensor_scalar(out=hn[:], in0=sap[:, hf * DH:(hf + 1) * DH], scalar1=ss[:, 2:3],
                                                     scalar2=None, op0=ALU.mult), reads=[r_src, r_ss], writes=[r_hn])
        for g0 in range(hf * KH, (hf + 1) * KH, G):
            for j in range(G):
                kl = g0 + j - hf * KH
                c.op("pe", lambda p, j=j, kl=kl: p.transpose(ptr[:, j, :], hn[:, kl * 128:(kl + 1) * 128], ident[:]),
                     reads=[r_hn, r_id], writes=[r_ptr], sig=(j == G - 1))
            gb = gain[:, g0:g0 + G].unsqueeze(2).to_broadcast([128, G, 128])
            if router is None:
                c.op("dve", lambda v, g0=g0, gb=gb: v.tensor_tensor(
                    out=xT[:, g0:g0 + G, tok0:tok0 + 128], in0=ptr[:, 0:G, :], in1=gb, op=ALU.mult),
                    reads=[r_ptr, r_gain], writes=[r_xT])
            else:
                xt32, r_xt32, rt, r_rt, plg, r_plg = router
                xi = gi_ % 2
                gi_ += 1
                c.op("dve", lambda v, gb=gb, xi=xi: v.tensor_tensor(out=xt32[xi][:, 0:G, :], in0=ptr[:, 0:G, :], in1=gb,
                                                                   op=ALU.mult), reads=[r_ptr, r_gain], writes=[r_xt32[xi]])
                c.op("act", lambda a, g0=g0, xi=xi: a.copy(out=xT[:, g0:g0 + G, tok0:tok0 + 128], in_=xt32[xi][:, 0:G, :]),
                     reads=[r_xt32[xi]], writes=[r_xT])
                for j in range(G):
                    kc = g0 + j
                    c.op("pe", lambda p, j=j, kc=kc, xi=xi: p.matmul(plg, lhsT=xt32[xi][:, j, :], rhs=rt[:, kc, :],
                                                                    start=(kc == 0), stop=(kc == KC - 1)),
                         reads=[r_xt32[xi], r_rt], writes=[r_plg], sig=(kc == KC - 1))
    return hn, r_hn, ss, r_ss


def alloc_norm(c, cfg, B, with_hs=True):
    D = cfg["D"]
    ident, r_id = make_ident(c)
    hs = c.sb("hs", [128, D], F32) if with_hs else None
    hn = c.sb("hn", [128, D // 2], F32)
    ss = c.sb("ss", [128, 8], F32)
    ptr = c.ps("ptr", [128, 4, 128], F32)
    B["norm"] = (hs, Reg(), hn, Reg(), ss, Reg(), ptr, Reg(), ident, r_id)


def load_const(c, name, dram_ap, shape, dt=F32, q="sp"):
    t = c.sb(name, shape, dt)
    r = Reg()
    c.dma(q, t[:], dram_ap, r, writes=[r])
    return t, r


def build_M(cfg, layer):
    D, KC, T, NST = cfg["D"], cfg["KC"], cfg["T"], cfg["NST"]
    nc = bass.Bass("TRN2", target_bir_lowering=False)
    c = Ctx(nc)

    def din(name, shape):
        return nc.dram_tensor(name, shape, F32, kind="ExternalInput").ap()

    def dout(name, shape):
        return nc.dram_tensor(name, shape, F32, kind="ExternalOutput").ap()

    hin = din("hin", [128 + T, D])
    gain_d = din("gain", [128, KC])
    wf_d = din("wf", [NFCH, 128, KC * 128])
    wv_d = din("wv", [12, 128, KC * 256])
    wup_d = din("wup", [16, 512])
    sm_d = din("smallp", [128, 96])
    wax_d = din("wax", [128, 16, 128])
    OL = dout("OL", [12, T, 257])
    QG = dout("QG", [12, 128, T])
    GT = dout("GT", [24, 128, T])
    RG1 = dout("RG1", [8, 128, T])
    RG2 = dout("RG2", [8, 128, T])
    STo = dout("STo", [128, 12, 257])
    SDo = dout("SDo", [128, 12])
    RGS = dout("RGS", [128, 16])

    B = {}
    alloc_norm(c, cfg, B)
    smallp, r_sm = load_const(c, "smallp", sm_d[:, :], [128, 96])
    gain, r_gain = load_const(c, "gain", gain_d[:, :], [128, KC])
    wup_b = c.sb("wup_b", [16, 512], BF16)
    r_wup = Reg()
    c.dma("pool", wup_b[:], wup_d[:, :], r_wup, writes=[r_wup])
    wax_b = c.sb("wax_b", [128, 16, 128], BF16)
    r_wax = Reg()
    c.dma("pool", wax_b[:], wax_d[:, :, :], r_wax, writes=[r_wax])

    der = c.sb("der", [128, 32], F32)
    r_der = Reg()
    c.op("act", lambda a: a.activation(out=der[:, 24:32], in_=smallp[:, 76:84], func=AF.Exp, scale=-1.0),
         reads=[r_sm], writes=[r_der])
    c.op("act", lambda a: a.activation(out=der[:, 24:32], in_=der[:, 24:32], func=AF.Ln, bias=1.0),
         reads=[r_der], writes=[r_der])
    c.op("dve", lambda v: v.tensor_scalar(out=der[:, 0:8], in0=der[:, 24:32], scalar1=-8.0, scalar2=None,
                                          op0=ALU.mult), reads=[r_der], writes=[r_der])
    if layer > 0:
        c.op("dve", lambda v: v.tensor_tensor(out=der[:, 12:16], in0=smallp[:, 16:20], in1=smallp[:, 12:16],
                                              op=ALU.subtract), reads=[r_sm, r_der], writes=[r_der])
        c.op("act", lambda a: a.activation(out=der[:, 8:12], in_=der[:, 12:16], func=AF.Sigmoid),
             reads=[r_der], writes=[r_der])
        c.op("dve", lambda v: v.tensor_scalar(out=der[:, 12:16], in0=der[:, 8:12], scalar1=-1.0, scalar2=1.0,
                                              op0=ALU.mult, op1=ALU.add), reads=[r_der], writes=[r_der])

    xT = c.sb("xT", [128, KC, 512], BF16)
    r_xT = Reg()
    xTh = c.sb("xTh", [128, KC, 128], BF16)
    r_xTh = Reg()

    NSLOT = 4
    wslot = [c.sb(f"wslot{i}", [128, KC, 256], BF16) for i in range(NSLOT)]
    r_wslot = [Reg() for _ in range(NSLOT)]
    wctr = [0]

    def load_w(dram_ap, ncols):
        i = wctr[0] % NSLOT
        wctr[0] += 1
        c.dma("pool", wslot[i][:, :, 0:ncols], dram_ap.rearrange("p (k n) -> p k n", n=ncols), r_wslot[i],
              writes=[r_wslot[i]])
        return wslot[i], r_wslot[i]

    pj = [c.ps(f"pj{i}", [128, 512], F32) for i in range(2)]
    r_pj = [Reg(), Reg()]
    pjc = [0]
    pv = c.ps("pv", [64, 2, 256], F32)
    _rpv = Reg()
    r_pv = [_rpv, _rpv]
    ptb = c.ps("ptb", [64, 8, 128], BF16)
    r_ptb = Reg()
    pmA = c.ps("pmA", [128, 512], F32)
    psc = pmA[0:64, 264:392].rearrange("p (a b) -> p a b", b=64)
    _rpm = Reg()
    r_psc = [_rpm, _rpm]
    po = [c.ps(f"po{i}", [64, 257], F32) for i in range(2)]
    r_po = [Reg(), Reg()]
    pu = pmA[:, 0:257]
    r_pu = _rpm

    def fbuf(name, n=512, dt=F32):
        return c.sb(name, [128, n], dt), Reg()

    qs, r_qs = fbuf("qs")
    kk, r_kk = fbuf("kk")
    la, r_la = fbuf("la")
    t1, r_t1 = fbuf("t1")
    t2, r_t2 = fbuf("t2")
    Bx, r_Bx = fbuf("Bx", 516)
    bl, r_bl = fbuf("bl")
    eq, r_eq = fbuf("eq")
    qt, r_qt = fbuf("qt", 512, BF16)
    kt, r_kt = fbuf("kt", 512, BF16)
    qg = [fbuf("qg0"), fbuf("qg1")]
    gts = [fbuf(f"gts{i}") for i in range(4)]
    gctr = [0]
    ones, r_ones = fbuf("ones")
    zeros, r_zeros = fbuf("zeros")
    c.op("pool", lambda g: g.memset(ones[:], 1.0), writes=[r_ones])
    c.op("pool", lambda g: g.memset(zeros[:], 0.0), writes=[r_zeros])
    lrT = c.sb("lrT", [16, 512], BF16)
    r_lrT = Reg()
    kT = c.sb("kT", [64, 8, 128], BF16)
    r_kT = Reg()
    Vb = c.sb("Vb", [64, 8, 257], BF16)
    r_Vb = Reg()
    c.op("pool", lambda g: g.memset(Vb[:], 1.0), writes=[r_Vb])
    maskT = c.sb("maskT", [64, 64], F32)
    r_mask = Reg()
    c.op("pool", lambda g: g.memset(maskT[:], 1.0), writes=[r_mask])
    c.op("pool", lambda g: g.affine_select(out=maskT[:], in_=maskT[:], pattern=[[1, 64]], compare_op=ALU.is_ge,
                                           fill=0.0, base=0, channel_multiplier=-1), reads=[r_mask], writes=[r_mask])
    AT = c.sb("AT", [64, 2, 64], BF16)
    r_AT = [Reg(), Reg()]
    S = c.sb("S", [128, 12, 257], F32)
    r_S = [Reg() for _ in range(12)]
    c.op("pool", lambda g: g.memset(S[:], 0.0), writes=r_S)
    Sb = c.sb("Sb", [128, 257], BF16)
    r_Sb = Reg()
    ut = c.sb("ut", [128, 257], F32)
    r_ut = Reg()
    osb = c.sb("osb", [64, 2, 257], F32)
    r_osb = [Reg(), Reg()]
    Bc = c.sb("Bc", [128, 12], F32)
    r_Bc = Reg()
    c.op("pool", lambda g: g.memset(Bc[:], 0.0), writes=[r_Bc])
    Xb, r_Xb = Bx, r_Bx
    Xc = c.sb("Xc", [128, 8, 4], F32)
    r_Xc = Reg()
    rgc = c.sb("rgc", [128, 16], F32)
    r_rgc = Reg()
    c.op("pool", lambda g: g.memset(rgc[:, 0:8], 0.0), writes=[r_rgc])
    c.op("pool", lambda g: g.memset(rgc[:, 8:16], 1.0), reads=[r_rgc], writes=[r_rgc])
    xc, r_xc = bl, r_bl
    xcb, r_xcb = qt, r_qt
    rgA, r_rgA = eq, r_eq
    rgU, r_rgU = qs, r_qs
    rgH, r_rgH = kk, r_kk
    rgG, r_rgG = la, r_la
    rgo = qg
    rgoc = [0]

    def proj_f(ci, rhs_ap=None, r_rhs=None, ncols=512, npart=128):
        w, rw = load_w(wf_d[ci], 128)
        i = pjc[0] % 2
        pjc[0] += 1
        rhs_t = xT if rhs_ap is None else rhs_ap
        rr = r_xT if r_rhs is None else r_rhs
        for kc in range(KC):
            c.op("pe", lambda p, kc=kc: p.matmul(pj[i][0:npart, 0:ncols], lhsT=w[:, kc, 0:npart],
                                                 rhs=rhs_t[:, kc, 0:ncols], start=(kc == 0), stop=(kc == KC - 1)),
                 reads=[rw, rr], writes=[r_pj[i]], sig=(kc == KC - 1))
        return pj[i], r_pj[i], w, rw

    def store_gate(pt, rp, func, gidx, tok0):
        g, rg = gts[gctr[0] % 4]
        gctr[0] += 1
        c.op("act", lambda a: a.activation(out=g[:], in_=pt[:, :], func=func), reads=[rp], writes=[rg])
        c.dma("sp", GT[gidx, :, tok0:tok0 + 512], g[:], rg, reads=[rg])

    def head_common(hh, tok0, last_st):
        c.op("dve", lambda v: v.tensor_copy(out=Bx[:, 0:1], in_=Bc[:, hh:hh + 1]), reads=[r_Bc], writes=[r_Bx])
        c.op("dve", lambda v: v.tensor_tensor_scan(out=Bx[:, 1:513], data0=ones[:], data1=la[:], initial=Bx[:, 0:1],
                                                   op0=ALU.mult, op1=ALU.add),
             reads=[r_Bx, r_ones, r_la], writes=[r_Bx])
        c.op("dve", lambda v: v.tensor_copy(out=Bc[:, hh:hh + 1], in_=Bx[:, 512:513]), reads=[r_Bx], writes=[r_Bc])
        c.op("dve", lambda v: v.tensor_tensor(
            out=bl[:].rearrange("p (c j) -> p c j", j=64),
            in0=Bx[:, 1:513].rearrange("p (c j) -> p c j", j=64),
            in1=Bx[:, 0:512].rearrange("p (c j) -> p c j", j=64)[:, :, 0:1].to_broadcast([128, 8, 64]),
            op=ALU.subtract), reads=[r_Bx], writes=[r_bl])
        c.op("act", lambda a: a.activation(out=eq[:], in_=bl[:], func=AF.Exp), reads=[r_bl], writes=[r_eq])
        c.op("act", lambda a: a.activation(out=t1[:], in_=bl[:], func=AF.Exp, scale=-1.0), reads=[r_bl], writes=[r_t1])
        c.op("act", lambda a: a.activation(out=t2[:], in_=Bx[:, 1:513], func=AF.Exp), reads=[r_Bx], writes=[r_t2])
        c.op("dve", lambda v: v.tensor_tensor(out=qt[:], in0=qs[:], in1=eq[:], op=ALU.mult),
             reads=[r_qs, r_eq], writes=[r_qt])
        c.op("dve", lambda v: v.tensor_tensor(out=kt[:], in0=kk[:], in1=t1[:], op=ALU.mult),
             reads=[r_kk, r_t1], writes=[r_kt])
        qgb, r_qgb = qg[hh % 2]
        c.op("dve", lambda v: v.tensor_tensor(out=qgb[:], in0=qs[:], in1=t2[:], op=ALU.mult),
             reads=[r_qs, r_t2], writes=[r_qgb])
        c.dma("sp", QG[hh, :, tok0:tok0 + 512], qgb[:], r_qgb, reads=[r_qgb])
        for cc in range(8):
            c.op("pe", lambda p, cc=cc: p.transpose(ptb[:, cc, :], kt[:, cc * 64:(cc + 1) * 64], B["identb"][:]),
                 reads=[r_kt, B["r_identb"]], writes=[r_ptb], sig=(cc == 7))
        c.op("act", lambda a: a.copy(out=kT[:], in_=ptb[:]), reads=[r_ptb], writes=[r_kT])
        wvt, r_wvt = load_w(wv_d[hh], 256)
        for cc in range(8):
            i = cc % 2
            for kc in range(KC):
                c.op("pe", lambda p, kc=kc, cc=cc, i=i: p.matmul(pv[:, i, :], lhsT=xT[:, kc, cc * 64:(cc + 1) * 64],
                                                                rhs=wvt[:, kc, 0:256], start=(kc == 0),
                                                                stop=(kc == KC - 1)),
                     reads=[r_xT, r_wvt], writes=[r_pv[i]], sig=(kc == KC - 1))
            c.op("act", lambda a, cc=cc, i=i: a.copy(out=Vb[:, cc, 0:256], in_=pv[:, i, :]),
                 reads=[r_pv[i]], writes=[r_Vb])
        eq3 = eq[:].rearrange("p (c j) -> p c j", j=64)
        for cc in range(8):
            i = cc % 2
            cs = slice(cc * 64, (cc + 1) * 64)
            c.op("pe", lambda p, i=i, cs=cs: p.matmul(psc[:, i, :], lhsT=kt[:, cs], rhs=qt[:, cs], start=True, stop=True),
                 reads=[r_kt, r_qt], writes=[r_psc[i]])
            c.op("dve", lambda v, i=i: v.tensor_tensor(out=AT[:, i, :], in0=psc[:, i, :], in1=maskT[:], op=ALU.mult),
                 reads=[r_psc[i], r_mask], writes=[r_AT[i]])
            c.op("act", lambda a: a.copy(out=Sb[:], in_=S[:, hh, :]), reads=[r_S[hh]], writes=[r_Sb])
            c.op("pe", lambda p, i=i, cc=cc: p.matmul(po[i][:, :], lhsT=AT[:, i, :], rhs=Vb[:, cc, :], start=True, stop=False),
                 reads=[r_AT[i], r_Vb], writes=[r_po[i]], sig=False)
            c.op("pe", lambda p, i=i, cs=cs: p.matmul(po[i][:, :], lhsT=qt[:, cs], rhs=Sb[:], start=False, stop=True),
                 reads=[r_qt, r_Sb], writes=[r_po[i]])
            c.op("act", lambda a, i=i: a.copy(out=osb[:, i, :], in_=po[i][:, :]), reads=[r_po[i]], writes=[r_osb[i]])
            c.dma("sp", OL[hh, tok0 + cc * 64: tok0 + cc * 64 + 64, :], osb[:, i, :], r_osb[i], reads=[r_osb[i]])
            c.op("pe", lambda p, cc=cc: p.matmul(pu[:, :], lhsT=kT[:, cc, :], rhs=Vb[:, cc, :], start=True, stop=True),
                 reads=[r_kT, r_Vb], writes=[r_pu])
            el = eq3[:, cc, 63:64]
            c.op("dve", lambda v, el=el: v.tensor_scalar(out=ut[:], in0=pu[:, :], scalar1=el, scalar2=None, op0=ALU.mult),
                 reads=[r_pu, r_eq], writes=[r_ut])
            c.op("dve", lambda v, el=el: v.scalar_tensor_tensor(out=S[:, hh, :], in0=S[:, hh, :], scalar=el, in1=ut[:],
                                                               op0=ALU.mult, op1=ALU.add),
                 reads=[r_S[hh], r_ut, r_eq], writes=[r_S[hh]])

    identb = c.sb("identb", [128, 128], BF16)
    r_identb = Reg()
    c.op("dve", lambda v: v.tensor_copy(out=identb[:], in_=B["norm"][8][:]), reads=[B["norm"][9]], writes=[r_identb])
    B["identb"], B["r_identb"] = identb, r_identb

    for st in range(NST):
        tok0 = st * 512
        last_st = st == NST - 1
        if st == 0:
            emit_norm_tile(c, cfg, B, hin[0:128, :], 0, 128, xTh, r_xTh, gain, r_gain)
        for tt in range(4):
            emit_norm_tile(c, cfg, B, hin[128 + tok0 + tt * 128: 128 + tok0 + (tt + 1) * 128, :], tt * 128, 128,
                           xT, r_xT, gain, r_gain)
        p, rp, _, _ = proj_f(0, npart=16)
        c.op("act", lambda a: a.copy(out=lrT[:], in_=p[0:16, :]), reads=[rp], writes=[r_lrT])
        for h in range(4):
            hh = h
            p, rp, _, _ = proj_f(1 + 4 * h)
            c.op("act", lambda a: a.activation(out=qs[:], in_=p[:, :], func=AF.Copy, scale=DKS), reads=[rp], writes=[r_qs])
            p, rp, _, _ = proj_f(2 + 4 * h)
            c.op("act", lambda a: a.copy(out=kk[:], in_=p[:, :]), reads=[rp], writes=[r_kk])
            i = pjc[0] % 2
            pjc[0] += 1
            c.op("pe", lambda p_, i=i, h=h: p_.matmul(pj[i][:, :], lhsT=wup_b[:, h * 128:(h + 1) * 128], rhs=lrT[:],
                                                     start=True, stop=True), reads=[r_wup, r_lrT], writes=[r_pj[i]])
            c.op("act", lambda a, i=i, h=h: a.activation(out=t1[:], in_=pj[i][:, :], func=AF.Sigmoid, bias=smallp[:, h:h + 1]),
                 reads=[r_pj[i], r_sm], writes=[r_t1])
            c.op("act", lambda a: a.activation(out=t1[:], in_=t1[:], func=AF.Ln), reads=[r_t1], writes=[r_t1])
            c.op("act", lambda a: a.mul(out=la[:], in_=t1[:], mul=1.0 / 16.0), reads=[r_t1], writes=[r_la])
            for half in range(2):
                p, rp, _, _ = proj_f(3 + 4 * h + half)
                store_gate(p, rp, AF.Silu, hh * 2 + half, tok0)
            head_common(hh, tok0, last_st)
        for n in range(8):
            p, rp, w, rw = proj_f(17 + 2 * n)
            c.op("act", lambda a: a.copy(out=Xb[:, 3:515], in_=p[:, :]), reads=[rp], writes=[r_Xb])
            if st == 0:
                i = pjc[0] % 2
                pjc[0] += 1
                for kc in range(KC):
                    c.op("pe", lambda p_, kc=kc, i=i: p_.matmul(pj[i][:, 0:128], lhsT=w[:, kc, 0:128], rhs=xTh[:, kc, :],
                                                               start=(kc == 0), stop=(kc == KC - 1)),
                         reads=[rw, r_xTh], writes=[r_pj[i]], sig=(kc == KC - 1))
                c.op("act", lambda a, i=i: a.copy(out=Xb[:, 0:3], in_=pj[i][:, 125:128]), reads=[r_pj[i]], writes=[r_Xb])
            else:
                c.op("dve", lambda v, n=n: v.tensor_copy(out=Xb[:, 0:3], in_=Xc[:, n, 0:3]), reads=[r_Xc], writes=[r_Xb])
            c.op("dve", lambda v, n=n: v.tensor_copy(out=Xc[:, n, 0:3], in_=Xb[:, 512:515]), reads=[r_Xb], writes=[r_Xc])
            cw = lambda j, n=n: smallp[:, 28 + n * 4 + j: 29 + n * 4 + j]
            c.op("dve", lambda v, n=n: v.tensor_scalar(out=xc[:], in0=Xb[:, 0:512], scalar1=cw(0), scalar2=smallp[:, 20 + n:21 + n],
                                                      op0=ALU.mult, op1=ALU.add), reads=[r_Xb, r_sm], writes=[r_xc])
            for j in range(1, 4):
                c.op("dve", lambda v, j=j: v.scalar_tensor_tensor(out=xc[:], in0=Xb[:, j:j + 512], scalar=cw(j), in1=xc[:],
                                                                 op0=ALU.mult, op1=ALU.add),
                     reads=[r_Xb, r_sm, r_xc], writes=[r_xc])
            c.op("act", lambda a: a.copy(out=xcb[:], in_=xc[:]), reads=[r_xc], writes=[r_xcb])
            i = pjc[0] % 2
            pjc[0] += 1
            c.op("pe", lambda p_, i=i, n=n: p_.matmul(pj[i][:, :], lhsT=wax_b[:, n, :], rhs=xcb[:], start=True, stop=True),
                 reads=[r_wax, r_xcb], writes=[r_pj[i]])
            c.op("act", lambda a, i=i, n=n: a.activation(out=t1[:], in_=pj[i][:, :], func=AF.Sigmoid, bias=smallp[:, 60 + n:61 + n]),
                 reads=[r_pj[i], r_sm], writes=[r_t1])
            i2 = pjc[0] % 2
            pjc[0] += 1
            c.op("pe", lambda p_, i2=i2, n=n: p_.matmul(pj[i2][:, :], lhsT=wax_b[:, 8 + n, :], rhs=xcb[:], start=True, stop=True),
                 reads=[r_wax, r_xcb], writes=[r_pj[i2]])
            c.op("act", lambda a, i2=i2, n=n: a.activation(out=t2[:], in_=pj[i2][:, :], func=AF.Sigmoid, bias=smallp[:, 68 + n:69 + n]),
                 reads=[r_pj[i2], r_sm], writes=[r_t2])
            c.op("act", lambda a, n=n: a.activation(out=rgA[:], in_=t1[:], func=AF.Exp, scale=der[:, n:n + 1]),
                 reads=[r_t1, r_der], writes=[r_rgA])
            c.op("dve", lambda v: v.tensor_tensor(out=rgU[:], in0=rgA[:], in1=rgA[:], op=ALU.mult), reads=[r_rgA], writes=[r_rgU])
            c.op("dve", lambda v: v.tensor_scalar(out=rgU[:], in0=rgU[:], scalar1=-1.0, scalar2=1.0, op0=ALU.mult, op1=ALU.add),
                 reads=[r_rgU], writes=[r_rgU])
            c.op("dve", lambda v: v.tensor_scalar(out=rgU[:], in0=rgU[:], scalar1=0.0, scalar2=None, op0=ALU.max),
                 reads=[r_rgU], writes=[r_rgU])
            c.op("act", lambda a: a.sqrt(out=rgU[:], in_=rgU[:]), reads=[r_rgU], writes=[r_rgU])
            c.op("dve", lambda v: v.tensor_tensor(out=t2[:], in0=t2[:], in1=xc[:], op=ALU.mult), reads=[r_t2, r_xc], writes=[r_t2])
            c.op("dve", lambda v: v.tensor_tensor(out=rgU[:], in0=rgU[:], in1=t2[:], op=ALU.mult), reads=[r_rgU, r_t2], writes=[r_rgU])
            c.op("dve", lambda v, n=n: v.tensor_tensor_scan(out=rgH[:], data0=rgA[:], data1=rgU[:], initial=rgc[:, n:n + 1],
                                                           op0=ALU.mult, op1=ALU.add),
                 reads=[r_rgA, r_rgU, r_rgc], writes=[r_rgH])
            c.op("dve", lambda v, n=n: v.tensor_tensor_scan(out=rgU[:], data0=rgA[:], data1=zeros[:], initial=rgc[:, 8 + n:9 + n],
                                                           op0=ALU.mult, op1=ALU.add),
                 reads=[r_rgA, r_zeros, r_rgc, r_rgU], writes=[r_rgU])
            c.op("dve", lambda v, n=n: v.tensor_copy(out=rgc[:, n:n + 1], in_=rgH[:, 511:512]), reads=[r_rgH, r_rgc], writes=[r_rgc])
            c.op("dve", lambda v, n=n: v.tensor_copy(out=rgc[:, 8 + n:9 + n], in_=rgU[:, 511:512]), reads=[r_rgU, r_rgc], writes=[r_rgc])
            p, rp, _, _ = proj_f(18 + 2 * n)
            c.op("act", lambda a: a.copy(out=rgG[:], in_=p[:, :]), reads=[rp], writes=[r_rgG])
            c.op("dve", lambda v: v.tensor_tensor(out=t1[:], in0=rgG[:], in1=rgG[:], op=ALU.mult), reads=[r_rgG], writes=[r_t1])
            c.op("dve", lambda v: v.tensor_scalar(out=t1[:], in0=t1[:], scalar1=0.044715, scalar2=1.0, op0=ALU.mult, op1=ALU.add),
                 reads=[r_t1], writes=[r_t1])
            c.op("dve", lambda v: v.tensor_tensor(out=t1[:], in0=t1[:], in1=rgG[:], op=ALU.mult), reads=[r_t1, r_rgG], writes=[r_t1])
            c.op("act", lambda a: a.activation(out=t1[:], in_=t1[:], func=AF.Sigmoid, scale=1.5957691216057308),
                 reads=[r_t1], writes=[r_t1])
            c.op("dve", lambda v: v.tensor_tensor(out=rgG[:], in0=rgG[:], in1=t1[:], op=ALU.mult), reads=[r_t1, r_rgG], writes=[r_rgG])
            o1, r_o1 = rgo[0]
            o2, r_o2 = rgo[1]
            c.op("dve", lambda v: v.tensor_tensor(out=o1[:], in0=rgG[:], in1=rgH[:], op=ALU.mult), reads=[r_rgG, r_rgH], writes=[r_o1])
            c.dma("sp", RG1[n, :, tok0:tok0 + 512], o1[:], r_o1, reads=[r_o1])
            c.op("dve", lambda v: v.tensor_tensor(out=o2[:], in0=rgG[:], in1=rgU[:], op=ALU.mult), reads=[r_rgG, r_rgU], writes=[r_o2])
            c.dma("sp", RG2[n, :, tok0:tok0 + 512], o2[:], r_o2, reads=[r_o2])
        for h in range(4):
            hh = 4 + h
            p, rp, _, _ = proj_f(33 + 6 * h)
            c.op("act", lambda a: a.activation(out=qs[:], in_=p[:, :], func=AF.Copy, scale=DKS), reads=[rp], writes=[r_qs])
            p, rp, _, _ = proj_f(33 + 6 * h + 4)
            c.op("act", lambda a, h=h: a.activation(out=t1[:], in_=p[:, :], func=AF.Exp, bias=smallp[:, 4 + h:5 + h]),
                 reads=[rp, r_sm], writes=[r_t1])
            p, rp, _, _ = proj_f(33 + 6 * h + 1)
            c.op("dve", lambda v: v.tensor_tensor(out=kk[:], in0=p[:, :], in1=t1[:], op=ALU.mult), reads=[rp, r_t1], writes=[r_kk])
            p, rp, _, _ = proj_f(33 + 6 * h + 5)
            c.op("act", lambda a, h=h: a.activation(out=t2[:], in_=p[:, :], func=AF.Sigmoid, bias=smallp[:, 8 + h:9 + h]),
                 reads=[rp, r_sm], writes=[r_t2])
            c.op("act", lambda a: a.activation(out=la[:], in_=t2[:], func=AF.Ln), reads=[r_t2], writes=[r_la])
            for half in range(2):
                p, rp, _, _ = proj_f(33 + 6 * h + 2 + half)
                store_gate(p, rp, AF.Sigmoid, hh * 2 + half, tok0)
            head_common(hh, tok0, last_st)
        for h in range(4):
            hh = 8 + h
            p, rp, _, _ = proj_f(57 + 4 * h)
            c.op("act", lambda a: a.activation(out=qs[:], in_=p[:, :], func=AF.Silu), reads=[rp], writes=[r_qs])
            c.op("act", lambda a: a.mul(out=qs[:], in_=qs[:], mul=DKS), reads=[r_qs], writes=[r_qs])
            p, rp, _, _ = proj_f(57 + 4 * h + 1)
            c.op("act", lambda a: a.activation(out=t1[:], in_=p[:, :], func=AF.Sigmoid), reads=[rp], writes=[r_t1])
            if layer > 0:
                c.op("dve", lambda v, h=h: v.tensor_scalar(out=t1[:], in0=t1[:], scalar1=der[:, 12 + h:13 + h],
                                                          scalar2=der[:, 8 + h:9 + h], op0=ALU.mult, op1=ALU.add),
                     reads=[r_t1, r_der], writes=[r_t1])
            c.op("act", lambda a: a.activation(out=la[:], in_=t1[:], func=AF.Ln), reads=[r_t1], writes=[r_la])
            c.op("dve", lambda v: v.tensor_scalar(out=kk[:], in0=t1[:], scalar1=-1.0, scalar2=1.0, op0=ALU.mult, op1=ALU.add),
                 reads=[r_t1], writes=[r_kk])
            for half in range(2):
                p, rp, _, _ = proj_f(57 + 4 * h + 2 + half)
                store_gate(p, rp, AF.Silu, hh * 2 + half, tok0)
            head_common(hh, tok0, last_st)

    r_all = Reg()
    c.dma("sp", STo[:, :, :], S[:], r_all, reads=r_S)
    c.op("act", lambda a: a.activation(out=Bc[:], in_=Bc[:], func=AF.Exp), reads=[r_Bc], writes=[r_Bc])
    c.dma("sp", SDo[:, :], Bc[:], r_Bc, reads=[r_Bc])
    c.dma("sp", RGS[:, :], rgc[:], r_rgc, reads=[r_rgc])
    c.finish()
    return nc


def build_R(cfg, layer, moe, final):
    D, KC, T, NST, NCT = cfg["D"], cfg["KC"], cfg["T"], cfg["NST"], cfg["NCT"]
    NE = cfg["NE"]
    NHC = cfg["NHE"] if moe else cfg["NH"]
    nc = bass.Bass("TRN2", target_bir_lowering=False)
    c = Ctx(nc)

    def din(name, shape):
        return nc.dram_tensor(name, shape, F32, kind="ExternalInput").ap()

    hin = din("hin", [T, D])
    OL = din("OL", [12, T, 257])
    QG = din("QG", [12, 128, T])
    GT = din("GT", [24, 128, T])
    RG1 = din("RG1", [8, 128, T])
    RG2 = din("RG2", [8, 128, T])
    PS = din("PS", [7, 128, 12 * 257])
    PD = din("PD", [7, 128, 12])
    PRG = din("PRG", [7, 128, 16])
    gmix_d = din("gmix", [128, 24])
    wo_d = din("wo", [NCT, 128, 32 * 256])
    g2_d = din("g2", [128, KC])
    g3_d = din("g3", [128, KC])
    if moe:
        w1_d = din("w1", [NE * NHC, 128, KC * 128])
        w3_d = din("w3", [NE * NHC, 128, KC * 128])
        w2_d = din("w2", [NE * NHC * 128, D])
        rt_d = din("router", [128, KC * 8])
    else:
        w1_d = din("w1", [NHC, 128, KC * 128])
        w3_d = din("w3", [NHC, 128, KC * 128])
        w2_d = din("w2", [NHC * 128, D])
    wg_d = din("wg", [NCT, 128, KC * 256])
    wp_d = din("wp", [128, 2 * D])
    pT_d = din("pT", [128, 2 * T])
    if final:
        gf_d = din("gf", [128, D])
    hout = nc.dram_tensor("hout", [T, D], F32, kind="ExternalOutput").ap()

    B = {}
    alloc_norm(c, cfg, B, with_hs=False)
    identb = c.sb("identb", [128, 128], BF16)
    r_identb = Reg()
    c.op("dve", lambda v: v.tensor_copy(out=identb[:], in_=B["norm"][8][:]), reads=[B["norm"][9]], writes=[r_identb])
    gmix, r_gmix = load_const(c, "gmix", gmix_d[:, :], [128, 24])
    g2, r_g2 = load_const(c, "g2", g2_d[:, :], [128, KC])
    g3, r_g3 = load_const(c, "g3", g3_d[:, :], [128, KC])
    if moe:
        rt = c.sb("rt", [128, KC, 8], F32)
        r_rt = Reg()
        c.dma("sp", rt[:], rt_d.rearrange("p (k n) -> p k n", n=8), r_rt, writes=[r_rt])

    S0b = c.sb("S0b", [128, 12, 257], BF16)
    r_S0b = Reg()
    h0 = c.sb("h0", [128, 8], F32)
    r_h0 = Reg()
    c.op("pool", lambda g: g.memset(h0[:], 0.0), writes=[r_h0])
    scope = ExitStack()
    S0 = scope.enter_context(nc.sbuf_tensor("sb_S0", [128, 12, 257], F32))
    stg = scope.enter_context(nc.sbuf_tensor("sb_stg", [128, 12, 257], F32))
    pdt = scope.enter_context(nc.sbuf_tensor("sb_pdt", [128, 12], F32))
    prg = scope.enter_context(nc.sbuf_tensor("sb_prg", [128, 16], F32))
    r_S0 = Reg()
    c.op("pool", lambda g: g.memset(S0[:], 0.0), writes=[r_S0])
    r_stg = Reg()
    r_pdt = Reg()
    r_prg = Reg()
    for j in range(7):
        c.dma("sp", stg[:], PS[j].rearrange("p (h v) -> p h v", v=257), r_stg, writes=[r_stg])
        c.dma("sp", pdt[:], PD[j], r_pdt, writes=[r_pdt])
        c.dma("sp", prg[:], PRG[j], r_prg, writes=[r_prg])
        for hh in range(12):
            c.op("dve", lambda v, hh=hh: v.scalar_tensor_tensor(out=S0[:, hh, :], in0=S0[:, hh, :], scalar=pdt[:, hh:hh + 1],
                                                               in1=stg[:, hh, :], op0=ALU.mult, op1=ALU.add),
                 reads=[r_S0, r_pdt, r_stg], writes=[r_S0])
        c.op("dve", lambda v: v.tensor_tensor(out=h0[:], in0=h0[:], in1=prg[:, 8:16], op=ALU.mult), reads=[r_h0, r_prg], writes=[r_h0])
        c.op("dve", lambda v: v.tensor_tensor(out=h0[:], in0=h0[:], in1=prg[:, 0:8], op=ALU.add), reads=[r_h0, r_prg], writes=[r_h0])
    c.op("act", lambda a: a.copy(out=S0b[:], in_=S0[:]), reads=[r_S0], writes=[r_S0b])
    for e in c.eng:
        c._waits(e, [r_S0b, r_h0], [])
    scope.close()

    XK = max(32, KC)
    xT = c.sb("xT", [128, XK, 512], BF16)
    r_xT = Reg()
    H = c.sb("H", [128, 4, D], F32)
    r_H = [Reg() for _ in range(4)]

    NSL = 2
    warena = [c.sb(f"wa{i}", [128, 12288], BF16) for i in range(NSL)]
    r_w1 = [Reg() for _ in range(NSL)]
    r_w3 = [Reg() for _ in range(NSL)]
    r_w2 = [Reg() for _ in range(NSL)]
    wc = [0]

    pmB = c.ps("pmB", [128, 512], F32)
    pc = pmB[:, 0:257]
    r_pc = Reg()
    plg = pmB[:, 264:272]
    r_plg = r_pc
    ptb = B["norm"][6]
    r_ptb = B["norm"][7]
    pa = [c.ps(f"pa{i}", [128, 512], F32) for i in range(2)]
    r_pa = [Reg(), Reg()]
    pb = [c.ps(f"pb{i}", [128, 512], F32) for i in range(2)]
    r_pb = [Reg(), Reg()]
    py = [c.ps(f"py{i}", [128, 512], F32) for i in range(2)]
    r_py = [Reg(), Reg()]
    pyc = [0]

    qgb = [c.sb(f"qgb{i}", [128, 128], BF16) for i in range(2)]
    r_qgb = [Reg(), Reg()]
    olt = [c.sb(f"olt{i}", [128, 257], F32) for i in range(2)]
    r_olt = [Reg(), Reg()]
    gtt = [c.sb(f"gtt{i}", [128, 2, 128], F32) for i in range(2)]
    r_gtt = [Reg(), Reg()]
    ot = c.sb("ot", [128, 257], F32)
    r_ot = Reg()
    oj = B["norm"][2][:, 0:256]
    r_oj = B["norm"][3]
    ob = c.sb("ob", [128, 256], F32)
    r_ob = Reg()
    sm = c.sb("sm", [128, 8], F32)
    r_smr = Reg()
    rg2 = [c.sb(f"rg2_{i}", [128, 512], F32) for i in range(2)]
    r_rg2 = [Reg(), Reg()]
    hid = [c.sb(f"hid{i}", [128, 512], BF16) for i in range(2)]
    r_hid = [Reg(), Reg()]
    sa = [c.sb(f"sa{i}", [128, 512], F32) for i in range(2)]
    r_sa = [Reg(), Reg()]
    rg1, r_rg1 = sa, r_sa
    wp_s = [c.sb(f"wp_s{i}", [128, 2, 256], BF16) for i in range(2)]
    r_wp_s = [Reg(), Reg()]
    pT_s = c.sb("pT_s", [128, 2, 512], BF16)
    r_pT_s = Reg()
    if final:
        gfs = c.sb("gfs", [128, D // 2], F32)
        r_gfs = Reg()
    comb = c.sb("comb", [128, 4, 8], F32)
    r_comb = [Reg() for _ in range(4)]
    lg = c.sb("lg", [128, 32], F32)
    r_lg = Reg()
    xt32 = [c.sb(f"xt32_{i}", [128, 4, 128], F32) for i in range(2)]
    r_xt32 = [Reg(), Reg()]

    def kc_of(hh, half):
        if hh < 4:
            return hh * 2 + half
        if hh < 8:
            return 16 + (hh - 4) * 2 + half
        return 24 + (hh - 8) * 2 + half

    for st in range(NST):
        tok0 = st * 512
        for tt in range(4):
            c.dma("sp", H[:, tt, :], hin[tok0 + tt * 128: tok0 + (tt + 1) * 128, :], r_H[tt], writes=[r_H[tt]])
        it = 0
        for hh in range(12):
            is_ml = 4 <= hh < 8
            for tt in range(4):
                i = it % 2
                it += 1
                t0 = tok0 + tt * 128
                c.dma("pool", qgb[i][:], QG[hh, :, t0:t0 + 128], r_qgb[i], writes=[r_qgb[i]])
                c.dma("sp", olt[i][:], OL[hh, t0:t0 + 128, :], r_olt[i], writes=[r_olt[i]])
                c.dma("sp", gtt[i][:], GT[hh * 2:hh * 2 + 2, :, t0:t0 + 128].rearrange("c p t -> p c t"), r_gtt[i],
                      writes=[r_gtt[i]])
                c.op("pe", lambda p, i=i, hh=hh: p.matmul(pc[:, :], lhsT=qgb[i][:], rhs=S0b[:, hh, :], start=True, stop=True),
                     reads=[r_qgb[i], r_S0b], writes=[r_pc])
                c.op("dve", lambda v, i=i: v.tensor_tensor(out=ot[:], in0=pc[:, :], in1=olt[i][:], op=ALU.add),
                     reads=[r_pc, r_olt[i]], writes=[r_ot])
                if is_ml:
                    c.op("act", lambda a: a.activation(out=sm[:, 6:7], in_=ot[:, 256:257], func=AF.Abs), reads=[r_ot], writes=[r_smr])
                    c.op("dve", lambda v: v.tensor_scalar(out=sm[:, 0:1], in0=sm[:, 6:7], scalar1=1.0, scalar2=None,
                                                          op0=ALU.max), reads=[r_smr], writes=[r_smr])
                    c.op("dve", lambda v: v.reciprocal(out=sm[:, 1:2], in_=sm[:, 0:1]), reads=[r_smr], writes=[r_smr])
                    c.op("act", lambda a: a.activation(out=oj, in_=ot[:, 0:256], func=AF.Square, scale=sm[:, 1:2],
                                                       accum_out=sm[:, 2:3]), reads=[r_ot, r_smr], writes=[r_oj, r_smr])
                else:
                    c.op("act", lambda a: a.activation(out=oj, in_=ot[:, 0:256], func=AF.Square, accum_out=sm[:, 2:3]),
                         reads=[r_ot], writes=[r_oj, r_smr])
                c.op("dve", lambda v: v.tensor_scalar(out=sm[:, 3:4], in0=sm[:, 2:3], scalar1=1.0 / 256, scalar2=EPS,
                                                      op0=ALU.mult, op1=ALU.add), reads=[r_smr], writes=[r_smr])
                c.op("act", lambda a: a.sqrt(out=sm[:, 5:6], in_=sm[:, 3:4]), reads=[r_smr], writes=[r_smr])
                c.op("dve", lambda v: v.reciprocal(out=sm[:, 4:5], in_=sm[:, 5:6]), reads=[r_smr], writes=[r_smr])
                if is_ml:
                    c.op("dve", lambda v: v.tensor_tensor(out=sm[:, 4:5], in0=sm[:, 4:5], in1=sm[:, 1:2], op=ALU.mult),
                         reads=[r_smr], writes=[r_smr])
                c.op("dve", lambda v: v.tensor_scalar(out=ob[:], in0=ot[:, 0:256], scalar1=sm[:, 4:5], scalar2=None, op0=ALU.mult),
                     reads=[r_ot, r_smr], writes=[r_ob])
                for half in range(2):
                    c.op("pe", lambda p, half=half: p.transpose(ptb[:, half, :], ob[:, half * 128:(half + 1) * 128], B["norm"][8][:]),
                         reads=[r_ob, B["norm"][9]], writes=[r_ptb], sig=(half == 1))
                for half in range(2):
                    gi = hh * 2 + half
                    c.op("dve", lambda v, half=half, gi=gi, i=i, tt=tt, hh=hh: v.scalar_tensor_tensor(
                        out=xT[:, kc_of(hh, half), tt * 128:(tt + 1) * 128], in0=ptb[:, half, :], scalar=gmix[:, gi:gi + 1],
                        in1=gtt[i][:, half, :], op0=ALU.mult, op1=ALU.mult),
                        reads=[r_ptb, r_gmix, r_gtt[i]], writes=[r_xT])
        for n in range(8):
            i = n % 2
            c.dma("sp", rg1[i][:], RG1[n, :, tok0:tok0 + 512], r_rg1[i], writes=[r_rg1[i]])
            c.dma("sp", rg2[i][:], RG2[n, :, tok0:tok0 + 512], r_rg2[i], writes=[r_rg2[i]])
            c.op("dve", lambda v, i=i, n=n: v.scalar_tensor_tensor(out=xT[:, 8 + n, :], in0=rg2[i][:], scalar=h0[:, n:n + 1],
                                                                  in1=rg1[i][:], op0=ALU.mult, op1=ALU.add),
                 reads=[r_rg1[i], r_rg2[i], r_h0], writes=[r_xT])
        for ct in range(NCT):
            s = wc[0] % NSL
            wc[0] += 1
            wt = warena[s][:, 0:32 * 256].rearrange("p (k n) -> p k n", n=256)
            c.dma("pool", wt, wo_d[ct].rearrange("p (k n) -> p k n", n=256), r_w1[s], writes=[r_w1[s], r_w3[s]])
            for tt in range(4):
                i = pyc[0] % 2
                pyc[0] += 1
                for kc in range(32):
                    c.op("pe", lambda p, kc=kc, i=i, tt=tt, wt=wt: p.matmul(py[i][:, 0:256], lhsT=xT[:, kc, tt * 128:(tt + 1) * 128],
                                                                          rhs=wt[:, kc, :], start=(kc == 0), stop=(kc == 31)),
                         reads=[r_xT, r_w1[s]], writes=[r_py[i]], sig=(kc == 31))
                c.op("dve", lambda v, i=i, tt=tt, ct=ct: v.tensor_tensor(out=H[:, tt, ct * 256:(ct + 1) * 256], in0=py[i][:, 0:256],
                                                                         in1=H[:, tt, ct * 256:(ct + 1) * 256], op=ALU.add),
                     reads=[r_py[i], r_H[tt]], writes=[r_H[tt]])
        for tt in range(4):
            if not moe:
                emit_norm_tile(c, cfg, B, None, tt * 128, 128, xT, r_xT, g2, r_g2, keep=(H[:, tt, :], r_H[tt]))
            else:
                emit_norm_tile(c, cfg, B, None, tt * 128, 128, xT, r_xT, g2, r_g2, keep=(H[:, tt, :], r_H[tt]),
                               router=(xt32, r_xt32, rt, r_rt, plg, r_plg))
                c.op("dve", lambda v: v.tensor_copy(out=lg[:, 0:8], in_=plg[:, :]), reads=[r_plg], writes=[r_lg])
                c.op("dve", lambda v: v.reduce_max(out=lg[:, 24:25], in_=lg[:, 0:8], axis=mybir.AxisListType.X), reads=[r_lg], writes=[r_lg])
                c.op("dve", lambda v: v.tensor_scalar(out=lg[:, 8:16], in0=lg[:, 0:8], scalar1=lg[:, 24:25], scalar2=None, op0=ALU.is_equal),
                     reads=[r_lg], writes=[r_lg])
                c.op("dve", lambda v: v.scalar_tensor_tensor(out=lg[:, 16:24], in0=lg[:, 8:16], scalar=-1e30, in1=lg[:, 0:8],
                                                             op0=ALU.mult, op1=ALU.add), reads=[r_lg], writes=[r_lg])
                c.op("dve", lambda v: v.reduce_max(out=lg[:, 25:26], in_=lg[:, 16:24], axis=mybir.AxisListType.X), reads=[r_lg], writes=[r_lg])
                c.op("dve", lambda v: v.tensor_scalar(out=lg[:, 16:24], in0=lg[:, 16:24], scalar1=lg[:, 25:26], scalar2=None, op0=ALU.is_equal),
                     reads=[r_lg], writes=[r_lg])
                c.op("dve", lambda v: v.tensor_tensor(out=lg[:, 26:27], in0=lg[:, 25:26], in1=lg[:, 24:25], op=ALU.subtract),
                     reads=[r_lg], writes=[r_lg])
                c.op("act", lambda a: a.activation(out=lg[:, 27:28], in_=lg[:, 26:27], func=AF.Exp), reads=[r_lg], writes=[r_lg])
                c.op("dve", lambda v: v.tensor_scalar(out=lg[:, 28:29], in0=lg[:, 27:28], scalar1=1.0, scalar2=None, op0=ALU.add),
                     reads=[r_lg], writes=[r_lg])
                c.op("dve", lambda v: v.reciprocal(out=lg[:, 29:30], in_=lg[:, 28:29]), reads=[r_lg], writes=[r_lg])
                c.op("dve", lambda v: v.tensor_tensor(out=lg[:, 30:31], in0=lg[:, 27:28], in1=lg[:, 29:30], op=ALU.mult),
                     reads=[r_lg], writes=[r_lg])
                c.op("dve", lambda v: v.tensor_scalar(out=lg[:, 8:16], in0=lg[:, 8:16], scalar1=lg[:, 29:30], scalar2=None, op0=ALU.mult),
                     reads=[r_lg], writes=[r_lg])
                c.op("dve", lambda v, tt=tt: v.scalar_tensor_tensor(out=comb[:, tt, :], in0=lg[:, 16:24], scalar=lg[:, 30:31],
                                                                   in1=lg[:, 8:16], op0=ALU.mult, op1=ALU.add),
                     reads=[r_lg], writes=[r_comb[tt]])
        for e in range(NE if moe else 1):
            for jc in range(NHC):
                cidx = e * NHC + jc
                s = wc[0] % NSL
                wc[0] += 1
                w1t = warena[s][:, 0:4096].rearrange("p (k n) -> p k n", n=128) if KC == 32 else \
                    warena[s][:, 0:KC * 128].rearrange("p (k n) -> p k n", n=128)
                w3t = warena[s][:, 4096:4096 + KC * 128].rearrange("p (k n) -> p k n", n=128)
                w2t = warena[s][:, 8192:8192 + D]
                c.dma("pool", w1t, w1_d[cidx].rearrange("p (k n) -> p k n", n=128), r_w1[s], writes=[r_w1[s]])
                c.dma("pool", w3t, w3_d[cidx].rearrange("p (k n) -> p k n", n=128), r_w3[s], writes=[r_w3[s]])
                c.dma("pool", w2t, w2_d[cidx * 128:(cidx + 1) * 128, :], r_w2[s], writes=[r_w2[s]])
                i = cidx % 2
                for kc in range(KC):
                    c.op("pe", lambda p, kc=kc, i=i, w1t=w1t: p.matmul(pa[i][:, :], lhsT=w1t[:, kc, :], rhs=xT[:, kc, :],
                                                                      start=(kc == 0), stop=(kc == KC - 1)),
                         reads=[r_w1[s], r_xT], writes=[r_pa[i]], sig=(kc == KC - 1))
                for kc in range(KC):
                    c.op("pe", lambda p, kc=kc, i=i, w3t=w3t: p.matmul(pb[i][:, :], lhsT=w3t[:, kc, :], rhs=xT[:, kc, :],
                                                                      start=(kc == 0), stop=(kc == KC - 1)),
                         reads=[r_w3[s], r_xT], writes=[r_pb[i]], sig=(kc == KC - 1))
                c.op("act", lambda a, i=i: a.activation(out=sa[i][:], in_=pa[i][:, :], func=AF.Silu), reads=[r_pa[i]], writes=[r_sa[i]])
                c.op("dve", lambda v, i=i: v.tensor_tensor(out=hid[i][:], in0=pb[i][:, :], in1=sa[i][:], op=ALU.mult),
                     reads=[r_pb[i], r_sa[i]], writes=[r_hid[i]])
                for tt in range(4):
                    for n0 in range(0, D, 512):
                        yi = pyc[0] % 2
                        pyc[0] += 1
                        c.op("pe", lambda p, yi=yi, i=i, tt=tt, n0=n0, w2t=w2t: p.matmul(
                            py[yi][:, :], lhsT=hid[i][:, tt * 128:(tt + 1) * 128], rhs=w2t[:, n0:n0 + 512], start=True, stop=True),
                            reads=[r_hid[i], r_w2[s]], writes=[r_py[yi]])
                        if moe:
                            c.op("dve", lambda v, yi=yi, tt=tt, n0=n0, e=e: v.scalar_tensor_tensor(
                                out=H[:, tt, n0:n0 + 512], in0=py[yi][:, :], scalar=comb[:, tt, e:e + 1], in1=H[:, tt, n0:n0 + 512],
                                op0=ALU.mult, op1=ALU.add), reads=[r_py[yi], r_comb[tt], r_H[tt]], writes=[r_H[tt]])
                        else:
                            c.op("dve", lambda v, yi=yi, tt=tt, n0=n0: v.tensor_tensor(
                                out=H[:, tt, n0:n0 + 512], in0=py[yi][:, :], in1=H[:, tt, n0:n0 + 512], op=ALU.add),
                                reads=[r_py[yi], r_H[tt]], writes=[r_H[tt]])
        for tt in range(4):
            emit_norm_tile(c, cfg, B, None, tt * 128, 128, xT, r_xT, g3, r_g3, keep=(H[:, tt, :], r_H[tt]))
        c.dma("pool", pT_s[:], pT_d.rearrange("p (k n) -> p k n", n=T)[:, :, tok0:tok0 + 512], r_pT_s, writes=[r_pT_s])
        for ct in range(NCT):
            s = wc[0] % NSL
            wc[0] += 1
            wt = warena[s][:, 0:KC * 256].rearrange("p (k n) -> p k n", n=256)
            c.dma("pool", wt, wg_d[ct].rearrange("p (k n) -> p k n", n=256), r_w1[s], writes=[r_w1[s], r_w3[s]])
            wi = ct % 2
            c.dma("pool", wp_s[wi][:], wp_d.rearrange("p (k n) -> p k n", n=D)[:, :, ct * 256:(ct + 1) * 256], r_wp_s[wi],
                  writes=[r_wp_s[wi]])
            for tt in range(4):
                i = (ct * 4 + tt) % 2
                for kc in range(KC):
                    c.op("pe", lambda p, kc=kc, i=i, tt=tt, wt=wt: p.matmul(pa[i][:, 0:256], lhsT=xT[:, kc, tt * 128:(tt + 1) * 128],
                                                                          rhs=wt[:, kc, :], start=(kc == 0), stop=(kc == KC - 1)),
                         reads=[r_xT, r_w1[s]], writes=[r_pa[i]], sig=(kc == KC - 1))
                for k2 in range(2):
                    c.op("pe", lambda p, k2=k2, i=i, tt=tt, ct=ct: p.matmul(
                        pb[i][:, 0:256], lhsT=pT_s[:, k2, tt * 128:(tt + 1) * 128],
                        rhs=wp_s[ct % 2][:, k2, :], start=(k2 == 0), stop=(k2 == 1)),
                        reads=[r_pT_s, r_wp_s[ct % 2]], writes=[r_pb[i]], sig=(k2 == 1))
                c.op("act", lambda a, i=i: a.activation(out=sa[i][:, 0:256], in_=pa[i][:, 0:256], func=AF.Sigmoid),
                     reads=[r_pa[i]], writes=[r_sa[i]])
                c.op("dve", lambda v, i=i: v.tensor_tensor(out=sa[i][:, 0:256], in0=pb[i][:, 0:256], in1=sa[i][:, 0:256], op=ALU.mult),
                     reads=[r_pb[i], r_sa[i]], writes=[r_sa[i]])
                c.op("dve", lambda v, i=i, tt=tt, ct=ct: v.tensor_tensor(out=H[:, tt, ct * 256:(ct + 1) * 256], in0=sa[i][:, 0:256],
                                                                         in1=H[:, tt, ct * 256:(ct + 1) * 256], op=ALU.add),
                     reads=[r_sa[i], r_H[tt]], writes=[r_H[tt]])
        for tt in range(4):
            t0 = tok0 + tt * 128
            if not final:
                c.dma("sp", hout[t0:t0 + 128, :], H[:, tt, :], r_H[tt], reads=[r_H[tt]])
            else:
                hs, r_hs, hn, r_hn, ss, r_ss, ptr, r_ptr, ident, r_id = B["norm"]
                sap = H[:, tt, :]
                DH = D // 2
                for hf in range(2):
                    c.op("act", lambda a, sap=sap, hf=hf: a.activation(out=hn[:], in_=sap[:, hf * DH:(hf + 1) * DH], func=AF.Square,
                                                                       accum_out=ss[:, 4 + hf:5 + hf]),
                         reads=[r_H[tt]], writes=[r_hn, r_ss])
                c.op("dve", lambda v: v.tensor_tensor(out=ss[:, 0:1], in0=ss[:, 4:5], in1=ss[:, 5:6], op=ALU.add),
                     reads=[r_ss], writes=[r_ss])
                c.op("dve", lambda v: v.tensor_scalar(out=ss[:, 1:2], in0=ss[:, 0:1], scalar1=1.0 / D, scalar2=EPS,
                                                      op0=ALU.mult, op1=ALU.add), reads=[r_ss], writes=[r_ss])
                c.op("act", lambda a: a.sqrt(out=ss[:, 3:4], in_=ss[:, 1:2]), reads=[r_ss], writes=[r_ss])
                c.op("dve", lambda v: v.reciprocal(out=ss[:, 2:3], in_=ss[:, 3:4]), reads=[r_ss], writes=[r_ss])
                for hf in range(2):
                    c.dma("sp", gfs[:], gf_d[:, hf * DH:(hf + 1) * DH], r_gfs, writes=[r_gfs])
                    c.op("dve", lambda v, sap=sap, hf=hf: v.scalar_tensor_tensor(out=hn[:], in0=sap[:, hf * DH:(hf + 1) * DH],
                                                                                scalar=ss[:, 2:3], in1=gfs[:],
                                                                                op0=ALU.mult, op1=ALU.mult),
                         reads=[r_H[tt], r_ss, r_gfs], writes=[r_hn])
                    c.dma("sp", hout[t0:t0 + 128, hf * DH:(hf + 1) * DH], hn[:], r_hn, reads=[r_hn])
    c.finish()
    return nc


def _pk(v, KC):
    return np.ascontiguousarray(np.asarray(v, np.float32).reshape(KC, 128).T)


def _chunkw(W, ncols):
    K, N = W.shape
    return np.ascontiguousarray(W.reshape(K // 128, 128, N // ncols, ncols).transpose(2, 1, 0, 3)).reshape(
        N // ncols, 128, (K // 128) * ncols)


_FC = fchunk_cols()


def _prep_M(cfg, l, inp):
    KC = cfg["KC"]
    w_in = np.asarray(inp["w_in"][l], np.float32)
    D = w_in.shape[0]
    wf = np.zeros((NFCH, 128, KC, 128), np.float32)
    wr = w_in.reshape(KC, 128, -1)
    for ci, cols in enumerate(_FC):
        ca = np.array(cols)
        ok = ca >= 0
        blk = np.zeros((KC, 128, 128), np.float32)
        blk[:, :, ok] = wr[:, :, ca[ok]]
        wf[ci] = blk.transpose(1, 0, 2)
    wf = wf.reshape(NFCH, 128, KC * 128)
    wv = np.zeros((12, 128, KC, 256), np.float32)
    for hh in range(12):
        wv[hh] = wr[:, :, V_OFF[hh]:V_OFF[hh] + 256].transpose(1, 0, 2)
    wv = wv.reshape(12, 128, KC * 256)
    sp = np.zeros((128, 96), np.float32)
    sp[:, 0:4] = np.asarray(inp["gla_b_up"][l]).reshape(4, 128).T
    sp[:, 4:8] = np.asarray(inp["ml_b_i"][l])[None, :]
    sp[:, 8:12] = np.asarray(inp["ml_b_f"][l])[None, :]
    sp[:, 12:16] = np.asarray(inp["hg_lb_logits"][0]).reshape(4, 128).T
    sp[:, 16:20] = np.asarray(inp["hg_lb_logits"][min(l, 1)]).reshape(4, 128).T if l > 0 else 0.0
    sp[:, 20:28] = np.asarray(inp["rg_conv_b"][l]).reshape(8, 128).T
    cw = np.asarray(inp["rg_conv_w"][l])
    sp[:, 28:60] = cw.reshape(4, 8, 128).transpose(2, 1, 0).reshape(128, 32)
    sp[:, 60:68] = np.asarray(inp["rg_b_a"][l]).reshape(8, 128).T
    sp[:, 68:76] = np.asarray(inp["rg_b_x"][l]).reshape(8, 128).T
    sp[:, 76:84] = np.asarray(inp["rg_lambda"][l]).reshape(8, 128).T
    wax = np.concatenate([np.asarray(inp["rg_w_a"][l]), np.asarray(inp["rg_w_x"][l])], axis=0)
    wax = np.ascontiguousarray(wax.transpose(1, 0, 2))
    return dict(gain=_pk(inp["attn_norm"][l], KC), wf=wf, wv=wv, wup=np.asarray(inp["gla_w_up"][l], np.float32),
                smallp=sp, wax=wax)


def _prep_R(cfg, l, inp, moe, final):
    KC, D = cfg["KC"], cfg["D"]
    j = l // 2
    d = {}
    gm = np.zeros((128, 24), np.float32)
    gl = np.asarray(inp["gla_norm"][l]).reshape(2, 128).T
    hg = np.asarray(inp["hg_norm"][l]).reshape(2, 128).T
    ml = np.asarray(inp["ml_norm"][l]).reshape(8, 128).T
    for h in range(4):
        gm[:, h * 2:h * 2 + 2] = gl
        gm[:, 8 + h * 2:8 + h * 2 + 2] = ml[:, h * 2:h * 2 + 2]
        gm[:, 16 + h * 2:16 + h * 2 + 2] = hg
    d["gmix"] = gm
    d["wo"] = _chunkw(np.asarray(inp["w_out"][l], np.float32), 256)
    d["g2"] = _pk(inp["ffn_norm"][l], KC)
    d["g3"] = _pk(inp["ple_norm"][l], KC)
    if moe:
        w1 = np.asarray(inp["moe_w1"][j], np.float32)
        w3 = np.asarray(inp["moe_w3"][j], np.float32)
        w2 = np.asarray(inp["moe_w2"][j], np.float32)
        d["w1"] = np.concatenate([_chunkw(w1[e], 128) for e in range(w1.shape[0])], axis=0)
        d["w3"] = np.concatenate([_chunkw(w3[e], 128) for e in range(w3.shape[0])], axis=0)
        d["w2"] = np.ascontiguousarray(w2.reshape(-1, D))
        r = np.asarray(inp["moe_router"][j], np.float32)
        d["router"] = np.ascontiguousarray(r.reshape(KC, 128, 8).transpose(1, 0, 2)).reshape(128, KC * 8)
    else:
        d["w1"] = _chunkw(np.asarray(inp["ffn_w1"][j], np.float32), 128)
        d["w3"] = _chunkw(np.asarray(inp["ffn_w3"][j], np.float32), 128)
        d["w2"] = np.ascontiguousarray(np.asarray(inp["ffn_w2"][j], np.float32))
    d["wg"] = _chunkw(np.asarray(inp["ple_w_gate"][l], np.float32), 256)
    wp = np.asarray(inp["ple_w_proj"][l], np.float32)
    d["wp"] = np.ascontiguousarray(wp.reshape(2, 128, D).transpose(1, 0, 2)).reshape(128, 2 * D)
    if final:
        d["gf"] = np.ascontiguousarray(np.broadcast_to(np.asarray(inp["final_norm"], np.float32)[None, :], (128, D)))
    return d


_PROG = {}


def _get_prog(kind, cfg, *args):
    key = (kind, tuple(sorted(cfg.items())), args)
    if key not in _PROG:
        _PROG[key] = build_M(cfg, *args) if kind == "M" else build_R(cfg, *args)
    return _PROG[key]


def run_model(cfg, inp):
    NCORE, T, D = cfg["NCORE"], cfg["T"], cfg["D"]
    x = np.asarray(inp["x"], np.float32).reshape(-1, D)
    p = np.asarray(inp["p"], np.float32)
    depth = p.shape[0]
    h = x
    cores = list(range(NCORE))
    for l in range(depth):
        moe = (l % 2 == 1)
        final = (l == depth - 1)
        wM = _prep_M(cfg, l, inp)
        maps = []
        for cidx in cores:
            halo = h[cidx * T - 128: cidx * T] if cidx > 0 else np.zeros((128, D), np.float32)
            m = dict(wM)
            m["hin"] = np.ascontiguousarray(np.concatenate([halo, h[cidx * T:(cidx + 1) * T]], axis=0))
            maps.append(m)
        resM = run_bass_kernel_spmd(_get_prog("M", cfg, l), maps, core_ids=cores).results
        del maps, wM
        wR = _prep_R(cfg, l, inp, moe, final)
        pT = np.ascontiguousarray(p[l].reshape(-1, 256).T)
        maps = []
        for cidx in cores:
            m = dict(wR)
            m["hin"] = np.ascontiguousarray(h[cidx * T:(cidx + 1) * T])
            for k in ("OL", "QG", "GT", "RG1", "RG2"):
                m[k] = resM[cidx][k]
            PS = np.zeros((7, 128, 12 * 257), np.float32)
            PD = np.ones((7, 128, 12), np.float32)
            PRG = np.zeros((7, 128, 16), np.float32)
            PRG[:, :, 8:16] = 1.0
            for jj in range(cidx):
                PS[jj] = resM[jj]["STo"].reshape(128, 12 * 257)
                PD[jj] = resM[jj]["SDo"]
                PRG[jj] = resM[jj]["RGS"]
            m["PS"], m["PD"], m["PRG"] = PS, PD, PRG
            pc_ = pT[:, cidx * T:(cidx + 1) * T]
            m["pT"] = np.ascontiguousarray(pc_.reshape(2, 128, T).transpose(1, 0, 2)).reshape(128, 2 * T)
            maps.append(m)
        resR = run_bass_kernel_spmd(_get_prog("R", cfg, l, moe, final), maps, core_ids=cores).results
        del maps, wR
        h = np.concatenate([resR[cidx]["hout"] for cidx in cores], axis=0)
    return h


def kernel(**inputs):
    cfg = make_cfg()
    x = inputs["x"]
    out = run_model(cfg, inputs)
    return out.reshape(x.shape).astype(np.float32, copy=False)
```

```python
from contextlib import ExitStack
import numpy as np
import concourse.bass as bass
import concourse.mybir as mybir
from concourse.bass_utils import run_bass_kernel_spmd

F32 = mybir.dt.float32
BF16 = mybir.dt.bfloat16
AF = mybir.ActivationFunctionType
ALU = mybir.AluOpType
EPS = 1e-6
SEM_MAX = 30000


class Reg:
    __slots__ = ("w", "r", "wpe", "ctr")

    def __init__(self):
        self.w = None
        self.r = {}
        self.wpe = False
        self.ctr = None


class Ctr:
    def __init__(self, ctx):
        self.c = ctx
        self.sems = []
        self.n = 0
        self.base = 0
        self.finals = []

    def _roll(self, inc):
        if not self.sems or self.n - self.base + inc > SEM_MAX:
            if self.sems:
                self.finals.append((self.sems[-1], self.n - self.base))
            self.sems.append(self.c.sem())
            self.base = self.n

    def bump(self, inc):
        self._roll(inc)
        self.n += inc
        return (self.sems[-1], self.n - self.base)

    def peek(self, inc):
        self._roll(inc)
        return (self.sems[-1], self.n - self.base + inc)


class Ctx:
    def __init__(self, nc):
        self.nc = nc
        self.es = ExitStack()
        self.eng = dict(pe=nc.tensor, act=nc.scalar, dve=nc.vector, pool=nc.gpsimd, sp=nc.sync)
        self.nsem = 0
        self.pc = {e: Ctr(self) for e in self.eng}
        self.known = {e: {} for e in self.eng}
        self.dctrs = []

    def sem(self):
        self.nsem += 1
        h = self.es.enter_context(self.nc.semaphore(f"s{self.nsem}"))
        return (h, self.nsem)

    def sb(self, name, shape, dt=F32):
        return self.es.enter_context(self.nc.sbuf_tensor("sb_" + name, shape, dt))

    def ps(self, name, shape, dt=F32):
        return self.es.enter_context(self.nc.psum_tensor("ps_" + name, shape, dt))

    def _waits(self, e, reads, writes):
        deps = {}

        def add(ev):
            if ev is None:
                return
            sm, v = ev
            if sm[1] not in deps or deps[sm[1]][1] < v:
                deps[sm[1]] = (sm, v)

        for r in reads:
            add(r.w)
        for w in writes:
            if not (e == "pe" and w.wpe):
                add(w.w)
            for ev in w.r.values():
                add(ev)
        kn = self.known[e]
        for sid, (sm, v) in deps.items():
            if kn.get(sid, 0) >= v:
                continue
            self.eng[e].wait_ge(sm[0], v)
            kn[sid] = v

    def op(self, e, fn, reads=(), writes=(), sig=True):
        self._waits(e, reads, writes)
        ins = fn(self.eng[e])
        if sig:
            ev = self.pc[e].bump(1)
            ins.then_inc(ev[0][0], 1)
        else:
            ev = self.pc[e].peek(1)
        for r in reads:
            r.r[ev[0][1]] = ev
        for w in writes:
            w.w = ev
            w.r = {}
            w.wpe = e == "pe"

    def dma(self, q, out, in_, sreg, reads=(), writes=()):
        self._waits(q, reads, writes)
        if sreg.ctr is None:
            sreg.ctr = Ctr(self)
            self.dctrs.append(sreg.ctr)
        ev = sreg.ctr.bump(16)
        self.eng[q].dma_start(out=out, in_=in_).then_inc(ev[0][0], 16)
        for r in reads:
            r.r[ev[0][1]] = ev
        for w in writes:
            w.w = ev
            w.r = {}
            w.wpe = False

    def finish(self):
        sp = self.eng["sp"]
        for ct in self.dctrs:
            for sm, v in ct.finals:
                sp.wait_ge(sm[0], v)
            if ct.sems:
                sp.wait_ge(ct.sems[-1][0], ct.n - ct.base)
        self.es.close()


def make_cfg(D=4096, T=2048, NCORE=8, DFF=11008, DFFE=5504, NE=8):
    return dict(D=D, KC=D // 128, T=T, NST=T // 512, NCORE=NCORE, DFF=DFF, DFFE=DFFE, NE=NE,
                NH=DFF // 128, NHE=DFFE // 128, NCT=D // 256)


O_GAQ, O_GAK, O_GAV, O_GAG, O_GALR = 0, 512, 1024, 2048, 3072
O_RGX, O_RGG = 3088, 4112
O_MLQ, O_MLK, O_MLV, O_MLO, O_MLI, O_MLF = 5136, 5648, 6160, 7184, 8208, 8212
O_HGQ, O_HGF, O_HGI, O_HGG = 8216, 8728, 9240, 10264
DKS = 128 ** -0.5

NFCH = 73


def fchunk_cols():
    cols = [list(range(O_GALR, O_GALR + 16)) + [-1] * 112]
    for h in range(4):
        cols.append(list(range(O_GAQ + 128 * h, O_GAQ + 128 * h + 128)))
        cols.append(list(range(O_GAK + 128 * h, O_GAK + 128 * h + 128)))
        cols.append(list(range(O_GAG + 256 * h, O_GAG + 256 * h + 128)))
        cols.append(list(range(O_GAG + 256 * h + 128, O_GAG + 256 * h + 256)))
    for n in range(8):
        cols.append(list(range(O_RGX + 128 * n, O_RGX + 128 * n + 128)))
        cols.append(list(range(O_RGG + 128 * n, O_RGG + 128 * n + 128)))
    for h in range(4):
        cols.append(list(range(O_MLQ + 128 * h, O_MLQ + 128 * h + 128)))
        cols.append(list(range(O_MLK + 128 * h, O_MLK + 128 * h + 128)))
        cols.append(list(range(O_MLO + 256 * h, O_MLO + 256 * h + 128)))
        cols.append(list(range(O_MLO + 256 * h + 128, O_MLO + 256 * h + 256)))
        cols.append([O_MLI + h] * 128)
        cols.append([O_MLF + h] * 128)
    for h in range(4):
        cols.append(list(range(O_HGQ + 128 * h, O_HGQ + 128 * h + 128)))
        cols.append(list(range(O_HGF + 128 * h, O_HGF + 128 * h + 128)))
        cols.append(list(range(O_HGG + 256 * h, O_HGG + 256 * h + 128)))
        cols.append(list(range(O_HGG + 256 * h + 128, O_HGG + 256 * h + 256)))
    assert len(cols) == NFCH
    return cols


V_OFF = [O_GAV + 256 * h for h in range(4)] + [O_MLV + 256 * h for h in range(4)] + [O_HGI + 256 * h for h in range(4)]


def make_ident(c, n=128):
    ident = c.sb("ident", [128, 128], F32)
    r = Reg()
    c.op("pool", lambda g: g.memset(ident[:], 1.0), writes=[r])
    c.op("pool", lambda g: g.affine_select(out=ident[:], in_=ident[:], pattern=[[-1, 128]],
                                           compare_op=ALU.is_equal, fill=0.0, base=0,
                                           channel_multiplier=1), reads=[r], writes=[r])
    return ident, r


def emit_norm_tile(c, cfg, B, src_ap, tok0, ntok_cols, xT, r_xT, gain, r_gain, rstd_out=None, keep=None, router=None):
    D, KC = cfg["D"], cfg["KC"]
    DH, KH = D // 2, KC // 2
    G = min(4, KH)
    hs, r_hs, hn, r_hn, ss, r_ss, ptr, r_ptr, ident, r_id = B["norm"]
    if keep is None:
        c.dma("sp", hs[:], src_ap, r_hs, writes=[r_hs])
        sap, r_src = hs[:], r_hs
    else:
        sap, r_src = keep
    for hf in range(2):
        c.op("act", lambda a, hf=hf: a.activation(out=hn[:], in_=sap[:, hf * DH:(hf + 1) * DH], func=AF.Square,
                                                  accum_out=ss[:, 4 + hf:5 + hf]),
             reads=[r_src], writes=[r_hn, r_ss])
    c.op("dve", lambda v: v.tensor_tensor(out=ss[:, 0:1], in0=ss[:, 4:5], in1=ss[:, 5:6], op=ALU.add),
         reads=[r_ss], writes=[r_ss])
    c.op("dve", lambda v: v.tensor_scalar(out=ss[:, 1:2], in0=ss[:, 0:1], scalar1=1.0 / D, scalar2=EPS,
                                          op0=ALU.mult, op1=ALU.add), reads=[r_ss], writes=[r_ss])
    c.op("act", lambda a: a.sqrt(out=ss[:, 3:4], in_=ss[:, 1:2]), reads=[r_ss], writes=[r_ss])
    c.op("dve", lambda v: v.reciprocal(out=ss[:, 2:3], in_=ss[:, 3:4]), reads=[r_ss], writes=[r_ss])
    gi_ = 0
    for hf in range(2):
        c.op("dve", lambda v, hf=hf: v.tensor_scalar(out=hn[:], in0=sap[:, hf * DH:(hf + 1) * DH], scalar1=ss[:, 2:3],
                                                     scalar2=None, op0=ALU.mult), reads=[r_src, r_ss], writes=[r_hn])
        for g0 in range(hf * KH, (hf + 1) * KH, G):
            for j in range(G):
                kl = g0 + j - hf * KH
                c.op("pe", lambda p, j=j, kl=kl: p.transpose(ptr[:, j, :], hn[:, kl * 128:(kl + 1) * 128], ident[:]),
                     reads=[r_hn, r_id], writes=[r_ptr], sig=(j == G - 1))
            gb = gain[:, g0:g0 + G].unsqueeze(2).to_broadcast([128, G, 128])
            if router is None:
                c.op("dve", lambda v, g0=g0, gb=gb: v.tensor_tensor(
                    out=xT[:, g0:g0 + G, tok0:tok0 + 128], in0=ptr[:, 0:G, :], in1=gb, op=ALU.mult),
                    reads=[r_ptr, r_gain], writes=[r_xT])
            else:
                xt32, r_xt32, rt, r_rt, plg, r_plg = router
                xi = gi_ % 2
                gi_ += 1
                c.op("dve", lambda v, gb=gb, xi=xi: v.tensor_tensor(out=xt32[xi][:, 0:G, :], in0=ptr[:, 0:G, :], in1=gb,
                                                                   op=ALU.mult), reads=[r_ptr, r_gain], writes=[r_xt32[xi]])
                c.op("act", lambda a, g0=g0, xi=xi: a.copy(out=xT[:, g0:g0 + G, tok0:tok0 + 128], in_=xt32[xi][:, 0:G, :]),
                     reads=[r_xt32[xi]], writes=[r_xT])
                for j in range(G):
                    kc = g0 + j
                    c.op("pe", lambda p, j=j, kc=kc, xi=xi: p.matmul(plg, lhsT=xt32[xi][:, j, :], rhs=rt[:, kc, :],
                                                                    start=(kc == 0), stop=(kc == KC - 1)),
                         reads=[r_xt32[xi], r_rt], writes=[r_plg], sig=(kc == KC - 1))
    return hn, r_hn, ss, r_ss


def alloc_norm(c, cfg, B, with_hs=True):
    D = cfg["D"]
    ident, r_id = make_ident(c)
    hs = c.sb("hs", [128, D], F32) if with_hs else None
    hn = c.sb("hn", [128, D // 2], F32)
    ss = c.sb("ss", [128, 8], F32)
    ptr = c.ps("ptr", [128, 4, 128], F32)
    B["norm"] = (hs, Reg(), hn, Reg(), ss, Reg(), ptr, Reg(), ident, r_id)


def load_const(c, name, dram_ap, shape, dt=F32, q="sp"):
    t = c.sb(name, shape, dt)
    r = Reg()
    c.dma(q, t[:], dram_ap, r, writes=[r])
    return t, r


def build_M(cfg, layer):
    D, KC, T, NST = cfg["D"], cfg["KC"], cfg["T"], cfg["NST"]
    nc = bass.Bass("TRN2", target_bir_lowering=False)
    c = Ctx(nc)

    def din(name, shape):
        return nc.dram_tensor(name, shape, F32, kind="ExternalInput").ap()

    def dout(name, shape):
        return nc.dram_tensor(name, shape, F32, kind="ExternalOutput").ap()

    hin = din("hin", [128 + T, D])
    gain_d = din("gain", [128, KC])
    wf_d = din("wf", [NFCH, 128, KC * 128])
    wv_d = din("wv", [12, 128, KC * 256])
    wup_d = din("wup", [16, 512])
    sm_d = din("smallp", [128, 96])
    wax_d = din("wax", [128, 16, 128])
    OL = dout("OL", [12, T, 257])
    QG = dout("QG", [12, 128, T])
    GT = dout("GT", [24, 128, T])
    RG1 = dout("RG1", [8, 128, T])
    RG2 = dout("RG2", [8, 128, T])
    STo = dout("STo", [128, 12, 257])
    SDo = dout("SDo", [128, 12])
    RGS = dout("RGS", [128, 16])

    B = {}
    alloc_norm(c, cfg, B)
    smallp, r_sm = load_const(c, "smallp", sm_d[:, :], [128, 96])
    gain, r_gain = load_const(c, "gain", gain_d[:, :], [128, KC])
    wup_b = c.sb("wup_b", [16, 512], BF16)
    r_wup = Reg()
    c.dma("pool", wup_b[:], wup_d[:, :], r_wup, writes=[r_wup])
    wax_b = c.sb("wax_b", [128, 16, 128], BF16)
    r_wax = Reg()
    c.dma("pool", wax_b[:], wax_d[:, :, :], r_wax, writes=[r_wax])

    der = c.sb("der", [128, 32], F32)
    r_der = Reg()
    c.op("act", lambda a: a.activation(out=der[:, 24:32], in_=smallp[:, 76:84], func=AF.Exp, scale=-1.0),
         reads=[r_sm], writes=[r_der])
    c.op("act", lambda a: a.activation(out=der[:, 24:32], in_=der[:, 24:32], func=AF.Ln, bias=1.0),
         reads=[r_der], writes=[r_der])
    c.op("dve", lambda v: v.tensor_scalar(out=der[:, 0:8], in0=der[:, 24:32], scalar1=-8.0, scalar2=None,
                                          op0=ALU.mult), reads=[r_der], writes=[r_der])
    if layer > 0:
        c.op("dve", lambda v: v.tensor_tensor(out=der[:, 12:16], in0=smallp[:, 16:20], in1=smallp[:, 12:16],
                                              op=ALU.subtract), reads=[r_sm, r_der], writes=[r_der])
        c.op("act", lambda a: a.activation(out=der[:, 8:12], in_=der[:, 12:16], func=AF.Sigmoid),
             reads=[r_der], writes=[r_der])
        c.op("dve", lambda v: v.tensor_scalar(out=der[:, 12:16], in0=der[:, 8:12], scalar1=-1.0, scalar2=1.0,
                                              op0=ALU.mult, op1=ALU.add), reads=[r_der], writes=[r_der])

    xT = c.sb("xT", [128, KC, 512], BF16)
    r_xT = Reg()
    xTh = c.sb("xTh", [128, KC, 128], BF16)
    r_xTh = Reg()

    NSLOT = 4
    wslot = [c.sb(f"wslot{i}", [128, KC, 256], BF16) for i in range(NSLOT)]
    r_wslot = [Reg() for _ in range(NSLOT)]
    wctr = [0]

    wseq = [("f", 0)]
    for h in range(4):
        wseq += [("f", 1 + 4 * h), ("f", 2 + 4 * h), ("f", 3 + 4 * h), ("f", 4 + 4 * h), ("v", h)]
    for n in range(8):
        wseq += [("f", 17 + 2 * n), ("f", 18 + 2 * n)]
    for h in range(4):
        b0 = 33 + 6 * h
        wseq += [("f", b0), ("f", b0 + 4), ("f", b0 + 1), ("f", b0 + 5), ("f", b0 + 2), ("f", b0 + 3), ("v", 4 + h)]
    for h in range(4):
        b0 = 57 + 4 * h
        wseq += [("f", b0), ("f", b0 + 1), ("f", b0 + 2), ("f", b0 + 3), ("v", 8 + h)]
    wseq_all = wseq * NST
    wpos = [0]
    wloaded = {}

    def _issue(g):
        kind, idx = wseq_all[g]
        i = g % NSLOT
        if kind == "f":
            c.dma("pool", wslot[i][:, :, 0:128], wf_d[idx].rearrange("p (k n) -> p k n", n=128), r_wslot[i],
                  writes=[r_wslot[i]])
        else:
            c.dma("pool", wslot[i][:, :, 0:256], wv_d[idx].rearrange("p (k n) -> p k n", n=256), r_wslot[i],
                  writes=[r_wslot[i]])
        wloaded[g] = (wslot[i], r_wslot[i])

    def load_w(kind, idx):
        g = wpos[0]
        assert wseq_all[g] == (kind, idx), (g, wseq_all[g], kind, idx)
        for gg in range(g, min(g + 3, len(wseq_all))):
            if gg not in wloaded:
                _issue(gg)
        wpos[0] += 1
        return wloaded.pop(g)

    pj = [c.ps(f"pj{i}", [128, 512], F32) for i in range(2)]
    r_pj = [Reg(), Reg()]
    pjc = [0]
    pv = c.ps("pv", [64, 2, 256], F32)
    _rpv = Reg()
    r_pv = [_rpv, _rpv]
    ptb = c.ps("ptb", [64, 8, 128], BF16)
    r_ptb = Reg()
    pmA = c.ps("pmA", [128, 512], F32)
    psc = pmA[0:64, 264:392].rearrange("p (a b) -> p a b", b=64)
    _rpm = Reg()
    r_psc = [_rpm, _rpm]
    po = [c.ps(f"po{i}", [64, 257], F32) for i in range(2)]
    r_po = [Reg(), Reg()]
    pu = pmA[:, 0:257]
    r_pu = _rpm

    def fbuf(name, n=512, dt=F32):
        return c.sb(name, [128, n], dt), Reg()

    qs, r_qs = fbuf("qs")
    kk, r_kk = fbuf("kk")
    la, r_la = fbuf("la")
    t1, r_t1 = fbuf("t1")
    t2, r_t2 = fbuf("t2")
    Bx, r_Bx = fbuf("Bx", 516)
    bl, r_bl = fbuf("bl")
    eq, r_eq = fbuf("eq")
    qt, r_qt = fbuf("qt", 512, BF16)
    kt, r_kt = fbuf("kt", 512, BF16)
    qg = [fbuf("qg0"), fbuf("qg1")]
    gts = [fbuf(f"gts{i}") for i in range(4)]
    gctr = [0]
    ones, r_ones = fbuf("ones")
    zeros, r_zeros = fbuf("zeros")
    c.op("pool", lambda g: g.memset(ones[:], 1.0), writes=[r_ones])
    c.op("pool", lambda g: g.memset(zeros[:], 0.0), writes=[r_zeros])
    lrT = c.sb("lrT", [16, 512], BF16)
    r_lrT = Reg()
    kT = c.sb("kT", [64, 8, 128], BF16)
    r_kT = Reg()
    Vb = c.sb("Vb", [64, 8, 257], BF16)
    r_Vb = Reg()
    c.op("pool", lambda g: g.memset(Vb[:], 1.0), writes=[r_Vb])
    maskT = c.sb("maskT", [64, 64], F32)
    r_mask = Reg()
    c.op("pool", lambda g: g.memset(maskT[:], 1.0), writes=[r_mask])
    c.op("pool", lambda g: g.affine_select(out=maskT[:], in_=maskT[:], pattern=[[1, 64]], compare_op=ALU.is_ge,
                                           fill=0.0, base=0, channel_multiplier=-1), reads=[r_mask], writes=[r_mask])
    AT = c.sb("AT", [64, 2, 64], BF16)
    r_AT = [Reg(), Reg()]
    S = c.sb("S", [128, 12, 257], F32)
    r_S = [Reg() for _ in range(12)]
    c.op("pool", lambda g: g.memset(S[:], 0.0), writes=r_S)
    Sb = c.sb("Sb", [128, 257], BF16)
    r_Sb = Reg()
    ut = c.sb("ut", [128, 257], F32)
    r_ut = Reg()
    osb = c.sb("osb", [64, 2, 257], F32)
    r_osb = [Reg(), Reg()]
    Bc = c.sb("Bc", [128, 12], F32)
    r_Bc = Reg()
    c.op("pool", lambda g: g.memset(Bc[:], 0.0), writes=[r_Bc])
    Xb, r_Xb = Bx, r_Bx
    Xc = c.sb("Xc", [128, 8, 4], F32)
    r_Xc = Reg()
    rgc = c.sb("rgc", [128, 16], F32)
    r_rgc = Reg()
    c.op("pool", lambda g: g.memset(rgc[:, 0:8], 0.0), writes=[r_rgc])
    c.op("pool", lambda g: g.memset(rgc[:, 8:16], 1.0), reads=[r_rgc], writes=[r_rgc])
    xc, r_xc = bl, r_bl
    xcb, r_xcb = qt, r_qt
    rgA, r_rgA = eq, r_eq
    rgU, r_rgU = qs, r_qs
    rgH, r_rgH = kk, r_kk
    rgG, r_rgG = la, r_la
    rgo = qg
    rgoc = [0]

    def proj_f(ci, rhs_ap=None, r_rhs=None, ncols=512, npart=128):
        w, rw = load_w("f", ci)
        i = pjc[0] % 2
        pjc[0] += 1
        rhs_t = xT if rhs_ap is None else rhs_ap
        rr = r_xT if r_rhs is None else r_rhs
        for kc in range(KC):
            c.op("pe", lambda p, kc=kc: p.matmul(pj[i][0:npart, 0:ncols], lhsT=w[:, kc, 0:npart],
                                                 rhs=rhs_t[:, kc, 0:ncols], start=(kc == 0), stop=(kc == KC - 1)),
                 reads=[rw, rr], writes=[r_pj[i]], sig=(kc == KC - 1))
        return pj[i], r_pj[i], w, rw

    def store_gate(pt, rp, func, gidx, tok0):
        g, rg = gts[gctr[0] % 4]
        gctr[0] += 1
        c.op("act", lambda a: a.activation(out=g[:], in_=pt[:, :], func=func), reads=[rp], writes=[rg])
        c.dma("sp", GT[gidx, :, tok0:tok0 + 512], g[:], rg, reads=[rg])

    def head_common(hh, tok0, last_st):
        c.op("dve", lambda v: v.tensor_copy(out=Bx[:, 0:1], in_=Bc[:, hh:hh + 1]), reads=[r_Bc], writes=[r_Bx])
        c.op("dve", lambda v: v.tensor_tensor_scan(out=Bx[:, 1:513], data0=ones[:], data1=la[:], initial=Bx[:, 0:1],
                                                   op0=ALU.mult, op1=ALU.add),
             reads=[r_Bx, r_ones, r_la], writes=[r_Bx])
        c.op("dve", lambda v: v.tensor_copy(out=Bc[:, hh:hh + 1], in_=Bx[:, 512:513]), reads=[r_Bx], writes=[r_Bc])
        c.op("dve", lambda v: v.tensor_tensor(
            out=bl[:].rearrange("p (c j) -> p c j", j=64),
            in0=Bx[:, 1:513].rearrange("p (c j) -> p c j", j=64),
            in1=Bx[:, 0:512].rearrange("p (c j) -> p c j", j=64)[:, :, 0:1].to_broadcast([128, 8, 64]),
            op=ALU.subtract), reads=[r_Bx], writes=[r_bl])
        c.op("act", lambda a: a.activation(out=eq[:], in_=bl[:], func=AF.Exp), reads=[r_bl], writes=[r_eq])
        c.op("act", lambda a: a.activation(out=t1[:], in_=bl[:], func=AF.Exp, scale=-1.0), reads=[r_bl], writes=[r_t1])
        c.op("act", lambda a: a.activation(out=t2[:], in_=Bx[:, 1:513], func=AF.Exp), reads=[r_Bx], writes=[r_t2])
        c.op("dve", lambda v: v.tensor_tensor(out=qt[:], in0=qs[:], in1=eq[:], op=ALU.mult),
             reads=[r_qs, r_eq], writes=[r_qt])
        c.op("dve", lambda v: v.tensor_tensor(out=kt[:], in0=kk[:], in1=t1[:], op=ALU.mult),
             reads=[r_kk, r_t1], writes=[r_kt])
        qgb, r_qgb = qg[hh % 2]
        c.op("dve", lambda v: v.tensor_tensor(out=qgb[:], in0=qs[:], in1=t2[:], op=ALU.mult),
             reads=[r_qs, r_t2], writes=[r_qgb])
        c.dma("sp", QG[hh, :, tok0:tok0 + 512], qgb[:], r_qgb, reads=[r_qgb])
        for cc in range(8):
            c.op("pe", lambda p, cc=cc: p.transpose(ptb[:, cc, :], kt[:, cc * 64:(cc + 1) * 64], B["identb"][:]),
                 reads=[r_kt, B["r_identb"]], writes=[r_ptb], sig=(cc == 7))
        c.op("act", lambda a: a.copy(out=kT[:], in_=ptb[:]), reads=[r_ptb], writes=[r_kT])
        wvt, r_wvt = load_w("v", hh)
        for cc in range(8):
            i = cc % 2
            for kc in range(KC):
                c.op("pe", lambda p, kc=kc, cc=cc, i=i: p.matmul(pv[:, i, :], lhsT=xT[:, kc, cc * 64:(cc + 1) * 64],
                                                                rhs=wvt[:, kc, 0:256], start=(kc == 0),
                                                                stop=(kc == KC - 1)),
                     reads=[r_xT, r_wvt], writes=[r_pv[i]], sig=(kc == KC - 1))
            c.op("act", lambda a, cc=cc, i=i: a.copy(out=Vb[:, cc, 0:256], in_=pv[:, i, :]),
                 reads=[r_pv[i]], writes=[r_Vb])
        eq3 = eq[:].rearrange("p (c j) -> p c j", j=64)
        for cc in range(8):
            i = cc % 2
            cs = slice(cc * 64, (cc + 1) * 64)
            c.op("pe", lambda p, i=i, cs=cs: p.matmul(psc[:, i, :], lhsT=kt[:, cs], rhs=qt[:, cs], start=True, stop=True),
                 reads=[r_kt, r_qt], writes=[r_psc[i]])
            c.op("dve", lambda v, i=i: v.tensor_tensor(out=AT[:, i, :], in0=psc[:, i, :], in1=maskT[:], op=ALU.mult),
                 reads=[r_psc[i], r_mask], writes=[r_AT[i]])
            c.op("act", lambda a: a.copy(out=Sb[:], in_=S[:, hh, :]), reads=[r_S[hh]], writes=[r_Sb])
            c.op("pe", lambda p, i=i, cc=cc: p.matmul(po[i][:, :], lhsT=AT[:, i, :], rhs=Vb[:, cc, :], start=True, stop=False),
                 reads=[r_AT[i], r_Vb], writes=[r_po[i]], sig=False)
            c.op("pe", lambda p, i=i, cs=cs: p.matmul(po[i][:, :], lhsT=qt[:, cs], rhs=Sb[:], start=False, stop=True),
                 reads=[r_qt, r_Sb], writes=[r_po[i]])
            c.op("act", lambda a, i=i: a.copy(out=osb[:, i, :], in_=po[i][:, :]), reads=[r_po[i]], writes=[r_osb[i]])
            c.dma("sp", OL[hh, tok0 + cc * 64: tok0 + cc * 64 + 64, :], osb[:, i, :], r_osb[i], reads=[r_osb[i]])
            c.op("pe", lambda p, cc=cc: p.matmul(pu[:, :], lhsT=kT[:, cc, :], rhs=Vb[:, cc, :], start=True, stop=True),
                 reads=[r_kT, r_Vb], writes=[r_pu])
            el = eq3[:, cc, 63:64]
            c.op("dve", lambda v, el=el: v.tensor_scalar(out=ut[:], in0=pu[:, :], scalar1=el, scalar2=None, op0=ALU.mult),
                 reads=[r_pu, r_eq], writes=[r_ut])
            c.op("dve", lambda v, el=el: v.scalar_tensor_tensor(out=S[:, hh, :], in0=S[:, hh, :], scalar=el, in1=ut[:],
                                                               op0=ALU.mult, op1=ALU.add),
                 reads=[r_S[hh], r_ut, r_eq], writes=[r_S[hh]])

    identb = c.sb("identb", [128, 128], BF16)
    r_identb = Reg()
    c.op("dve", lambda v: v.tensor_copy(out=identb[:], in_=B["norm"][8][:]), reads=[B["norm"][9]], writes=[r_identb])
    B["identb"], B["r_identb"] = identb, r_identb

    for st in range(NST):
        tok0 = st * 512
        last_st = st == NST - 1
        if st == 0:
            emit_norm_tile(c, cfg, B, hin[0:128, :], 0, 128, xTh, r_xTh, gain, r_gain)
        for tt in range(4):
            emit_norm_tile(c, cfg, B, hin[128 + tok0 + tt * 128: 128 + tok0 + (tt + 1) * 128, :], tt * 128, 128,
                           xT, r_xT, gain, r_gain)
        p, rp, _, _ = proj_f(0, npart=16)
        c.op("act", lambda a: a.copy(out=lrT[:], in_=p[0:16, :]), reads=[rp], writes=[r_lrT])
        for h in range(4):
            hh = h
            p, rp, _, _ = proj_f(1 + 4 * h)
            c.op("act", lambda a: a.activation(out=qs[:], in_=p[:, :], func=AF.Copy, scale=DKS), reads=[rp], writes=[r_qs])
            p, rp, _, _ = proj_f(2 + 4 * h)
            c.op("act", lambda a: a.copy(out=kk[:], in_=p[:, :]), reads=[rp], writes=[r_kk])
            i = pjc[0] % 2
            pjc[0] += 1
            c.op("pe", lambda p_, i=i, h=h: p_.matmul(pj[i][:, :], lhsT=wup_b[:, h * 128:(h + 1) * 128], rhs=lrT[:],
                                                     start=True, stop=True), reads=[r_wup, r_lrT], writes=[r_pj[i]])
            c.op("act", lambda a, i=i, h=h: a.activation(out=t1[:], in_=pj[i][:, :], func=AF.Sigmoid, bias=smallp[:, h:h + 1]),
                 reads=[r_pj[i], r_sm], writes=[r_t1])
            c.op("act", lambda a: a.activation(out=t1[:], in_=t1[:], func=AF.Ln), reads=[r_t1], writes=[r_t1])
            c.op("act", lambda a: a.mul(out=la[:], in_=t1[:], mul=1.0 / 16.0), reads=[r_t1], writes=[r_la])
            for half in range(2):
                p, rp, _, _ = proj_f(3 + 4 * h + half)
                store_gate(p, rp, AF.Silu, hh * 2 + half, tok0)
            head_common(hh, tok0, last_st)
        for n in range(8):
            p, rp, w, rw = proj_f(17 + 2 * n)
            c.op("act", lambda a: a.copy(out=Xb[:, 3:515], in_=p[:, :]), reads=[rp], writes=[r_Xb])
            if st == 0:
                i = pjc[0] % 2
                pjc[0] += 1
                for kc in range(KC):
                    c.op("pe", lambda p_, kc=kc, i=i: p_.matmul(pj[i][:, 0:128], lhsT=w[:, kc, 0:128], rhs=xTh[:, kc, :],
                                                               start=(kc == 0), stop=(kc == KC - 1)),
                         reads=[rw, r_xTh], writes=[r_pj[i]], sig=(kc == KC - 1))
                c.op("act", lambda a, i=i: a.copy(out=Xb[:, 0:3], in_=pj[i][:, 125:128]), reads=[r_pj[i]], writes=[r_Xb])
            else:
                c.op("dve", lambda v, n=n: v.tensor_copy(out=Xb[:, 0:3], in_=Xc[:, n, 0:3]), reads=[r_Xc], writes=[r_Xb])
            c.op("dve", lambda v, n=n: v.tensor_copy(out=Xc[:, n, 0:3], in_=Xb[:, 512:515]), reads=[r_Xb], writes=[r_Xc])
            cw = lambda j, n=n: smallp[:, 28 + n * 4 + j: 29 + n * 4 + j]
            c.op("dve", lambda v, n=n: v.tensor_scalar(out=xc[:], in0=Xb[:, 0:512], scalar1=cw(0), scalar2=smallp[:, 20 + n:21 + n],
                                                      op0=ALU.mult, op1=ALU.add), reads=[r_Xb, r_sm], writes=[r_xc])
            for j in range(1, 4):
                c.op("dve", lambda v, j=j: v.scalar_tensor_tensor(out=xc[:], in0=Xb[:, j:j + 512], scalar=cw(j), in1=xc[:],
                                                                 op0=ALU.mult, op1=ALU.add),
                     reads=[r_Xb, r_sm, r_xc], writes=[r_xc])
            c.op("act", lambda a: a.copy(out=xcb[:], in_=xc[:]), reads=[r_xc], writes=[r_xcb])
            i = pjc[0] % 2
            pjc[0] += 1
            c.op("pe", lambda p_, i=i, n=n: p_.matmul(pj[i][:, :], lhsT=wax_b[:, n, :], rhs=xcb[:], start=True, stop=True),
                 reads=[r_wax, r_xcb], writes=[r_pj[i]])
            c.op("act", lambda a, i=i, n=n: a.activation(out=t1[:], in_=pj[i][:, :], func=AF.Sigmoid, bias=smallp[:, 60 + n:61 + n]),
                 reads=[r_pj[i], r_sm], writes=[r_t1])
            i2 = pjc[0] % 2
            pjc[0] += 1
            c.op("pe", lambda p_, i2=i2, n=n: p_.matmul(pj[i2][:, :], lhsT=wax_b[:, 8 + n, :], rhs=xcb[:], start=True, stop=True),
                 reads=[r_wax, r_xcb], writes=[r_pj[i2]])
            c.op("act", lambda a, i2=i2, n=n: a.activation(out=t2[:], in_=pj[i2][:, :], func=AF.Sigmoid, bias=smallp[:, 68 + n:69 + n]),
                 reads=[r_pj[i2], r_sm], writes=[r_t2])
            c.op("act", lambda a, n=n: a.activation(out=rgA[:], in_=t1[:], func=AF.Exp, scale=der[:, n:n + 1]),
                 reads=[r_t1, r_der], writes=[r_rgA])
            c.op("dve", lambda v: v.tensor_tensor(out=rgU[:], in0=rgA[:], in1=rgA[:], op=ALU.mult), reads=[r_rgA], writes=[r_rgU])
            c.op("dve", lambda v: v.tensor_scalar(out=rgU[:], in0=rgU[:], scalar1=-1.0, scalar2=1.0, op0=ALU.mult, op1=ALU.add),
                 reads=[r_rgU], writes=[r_rgU])
            c.op("dve", lambda v: v.tensor_scalar(out=rgU[:], in0=rgU[:], scalar1=0.0, scalar2=None, op0=ALU.max),
                 reads=[r_rgU], writes=[r_rgU])
            c.op("act", lambda a: a.sqrt(out=rgU[:], in_=rgU[:]), reads=[r_rgU], writes=[r_rgU])
            c.op("dve", lambda v: v.tensor_tensor(out=t2[:], in0=t2[:], in1=xc[:], op=ALU.mult), reads=[r_t2, r_xc], writes=[r_t2])
            c.op("dve", lambda v: v.tensor_tensor(out=rgU[:], in0=rgU[:], in1=t2[:], op=ALU.mult), reads=[r_rgU, r_t2], writes=[r_rgU])
            c.op("dve", lambda v, n=n: v.tensor_tensor_scan(out=rgH[:], data0=rgA[:], data1=rgU[:], initial=rgc[:, n:n + 1],
                                                           op0=ALU.mult, op1=ALU.add),
                 reads=[r_rgA, r_rgU, r_rgc], writes=[r_rgH])
            c.op("dve", lambda v, n=n: v.tensor_tensor_scan(out=rgU[:], data0=rgA[:], data1=zeros[:], initial=rgc[:, 8 + n:9 + n],
                                                           op0=ALU.mult, op1=ALU.add),
                 reads=[r_rgA, r_zeros, r_rgc, r_rgU], writes=[r_rgU])
            c.op("dve", lambda v, n=n: v.tensor_copy(out=rgc[:, n:n + 1], in_=rgH[:, 511:512]), reads=[r_rgH, r_rgc], writes=[r_rgc])
            c.op("dve", lambda v, n=n: v.tensor_copy(out=rgc[:, 8 + n:9 + n], in_=rgU[:, 511:512]), reads=[r_rgU, r_rgc], writes=[r_rgc])
            p, rp, _, _ = proj_f(18 + 2 * n)
            c.op("act", lambda a: a.copy(out=rgG[:], in_=p[:, :]), reads=[rp], writes=[r_rgG])
            c.op("dve", lambda v: v.tensor_tensor(out=t1[:], in0=rgG[:], in1=rgG[:], op=ALU.mult), reads=[r_rgG], writes=[r_t1])
            c.op("dve", lambda v: v.tensor_scalar(out=t1[:], in0=t1[:], scalar1=0.044715, scalar2=1.0, op0=ALU.mult, op1=ALU.add),
                 reads=[r_t1], writes=[r_t1])
            c.op("dve", lambda v: v.tensor_tensor(out=t1[:], in0=t1[:], in1=rgG[:], op=ALU.mult), reads=[r_t1, r_rgG], writes=[r_t1])
            c.op("act", lambda a: a.activation(out=t1[:], in_=t1[:], func=AF.Sigmoid, scale=1.5957691216057308),
                 reads=[r_t1], writes=[r_t1])
            c.op("dve", lambda v: v.tensor_tensor(out=rgG[:], in0=rgG[:], in1=t1[:], op=ALU.mult), reads=[r_t1, r_rgG], writes=[r_rgG])
            o1, r_o1 = rgo[0]
            o2, r_o2 = rgo[1]
            c.op("dve", lambda v: v.tensor_tensor(out=o1[:], in0=rgG[:], in1=rgH[:], op=ALU.mult), reads=[r_rgG, r_rgH], writes=[r_o1])
            c.dma("sp", RG1[n, :, tok0:tok0 + 512], o1[:], r_o1, reads=[r_o1])
            c.op("dve", lambda v: v.tensor_tensor(out=o2[:], in0=rgG[:], in1=rgU[:], op=ALU.mult), reads=[r_rgG, r_rgU], writes=[r_o2])
            c.dma("sp", RG2[n, :, tok0:tok0 + 512], o2[:], r_o2, reads=[r_o2])
        for h in range(4):
            hh = 4 + h
            p, rp, _, _ = proj_f(33 + 6 * h)
            c.op("act", lambda a: a.activation(out=qs[:], in_=p[:, :], func=AF.Copy, scale=DKS), reads=[rp], writes=[r_qs])
            p, rp, _, _ = proj_f(33 + 6 * h + 4)
            c.op("act", lambda a, h=h: a.activation(out=t1[:], in_=p[:, :], func=AF.Exp, bias=smallp[:, 4 + h:5 + h]),
                 reads=[rp, r_sm], writes=[r_t1])
            p, rp, _, _ = proj_f(33 + 6 * h + 1)
            c.op("dve", lambda v: v.tensor_tensor(out=kk[:], in0=p[:, :], in1=t1[:], op=ALU.mult), reads=[rp, r_t1], writes=[r_kk])
            p, rp, _, _ = proj_f(33 + 6 * h + 5)
            c.op("act", lambda a, h=h: a.activation(out=t2[:], in_=p[:, :], func=AF.Sigmoid, bias=smallp[:, 8 + h:9 + h]),
                 reads=[rp, r_sm], writes=[r_t2])
            c.op("act", lambda a: a.activation(out=la[:], in_=t2[:], func=AF.Ln), reads=[r_t2], writes=[r_la])
            for half in range(2):
                p, rp, _, _ = proj_f(33 + 6 * h + 2 + half)
                store_gate(p, rp, AF.Sigmoid, hh * 2 + half, tok0)
            head_common(hh, tok0, last_st)
        for h in range(4):
            hh = 8 + h
            p, rp, _, _ = proj_f(57 + 4 * h)
            c.op("act", lambda a: a.activation(out=qs[:], in_=p[:, :], func=AF.Silu), reads=[rp], writes=[r_qs])
            c.op("act", lambda a: a.mul(out=qs[:], in_=qs[:], mul=DKS), reads=[r_qs], writes=[r_qs])
            p, rp, _, _ = proj_f(57 + 4 * h + 1)
            c.op("act", lambda a: a.activation(out=t1[:], in_=p[:, :], func=AF.Sigmoid), reads=[rp], writes=[r_t1])
            if layer > 0:
                c.op("dve", lambda v, h=h: v.tensor_scalar(out=t1[:], in0=t1[:], scalar1=der[:, 12 + h:13 + h],
                                                          scalar2=der[:, 8 + h:9 + h], op0=ALU.mult, op1=ALU.add),
                     reads=[r_t1, r_der], writes=[r_t1])
            c.op("act", lambda a: a.activation(out=la[:], in_=t1[:], func=AF.Ln), reads=[r_t1], writes=[r_la])
            c.op("dve", lambda v: v.tensor_scalar(out=kk[:], in0=t1[:], scalar1=-1.0, scalar2=1.0, op0=ALU.mult, op1=ALU.add),
                 reads=[r_t1], writes=[r_kk])
            for half in range(2):
                p, rp, _, _ = proj_f(57 + 4 * h + 2 + half)
                store_gate(p, rp, AF.Silu, hh * 2 + half, tok0)
            head_common(hh, tok0, last_st)

    r_all = Reg()
    c.dma("sp", STo[:, :, :], S[:], r_all, reads=r_S)
    c.op("act", lambda a: a.activation(out=Bc[:], in_=Bc[:], func=AF.Exp), reads=[r_Bc], writes=[r_Bc])
    c.dma("sp", SDo[:, :], Bc[:], r_Bc, reads=[r_Bc])
    c.dma("sp", RGS[:, :], rgc[:], r_rgc, reads=[r_rgc])
    c.finish()
    return nc


def build_R(cfg, layer, moe, final):
    D, KC, T, NST, NCT = cfg["D"], cfg["KC"], cfg["T"], cfg["NST"], cfg["NCT"]
    NE = cfg["NE"]
    NHC = cfg["NHE"] if moe else cfg["NH"]
    nc = bass.Bass("TRN2", target_bir_lowering=False)
    c = Ctx(nc)

    def din(name, shape):
        return nc.dram_tensor(name, shape, F32, kind="ExternalInput").ap()

    hin = din("hin", [T, D])
    OL = din("OL", [12, T, 257])
    QG = din("QG", [12, 128, T])
    GT = din("GT", [24, 128, T])
    RG1 = din("RG1", [8, 128, T])
    RG2 = din("RG2", [8, 128, T])
    PS = din("PS", [7, 128, 12 * 257])
    PD = din("PD", [7, 128, 12])
    PRG = din("PRG", [7, 128, 16])
    gmix_d = din("gmix", [128, 24])
    wo_d = din("wo", [NCT, 128, 32 * 256])
    g2_d = din("g2", [128, KC])
    g3_d = din("g3", [128, KC])
    if moe:
        w1_d = din("w1", [NE * NHC, 128, KC * 128])
        w3_d = din("w3", [NE * NHC, 128, KC * 128])
        w2_d = din("w2", [NE * NHC * 128, D])
        rt_d = din("router", [128, KC * 8])
    else:
        w1_d = din("w1", [NHC, 128, KC * 128])
        w3_d = din("w3", [NHC, 128, KC * 128])
        w2_d = din("w2", [NHC * 128, D])
    wg_d = din("wg", [NCT, 128, KC * 256])
    wp_d = din("wp", [128, 2 * D])
    pT_d = din("pT", [128, 2 * T])
    if final:
        gf_d = din("gf", [128, D])
    hout = nc.dram_tensor("hout", [T, D], F32, kind="ExternalOutput").ap()

    B = {}
    alloc_norm(c, cfg, B, with_hs=False)
    identb = c.sb("identb", [128, 128], BF16)
    r_identb = Reg()
    c.op("dve", lambda v: v.tensor_copy(out=identb[:], in_=B["norm"][8][:]), reads=[B["norm"][9]], writes=[r_identb])
    gmix, r_gmix = load_const(c, "gmix", gmix_d[:, :], [128, 24])
    g2, r_g2 = load_const(c, "g2", g2_d[:, :], [128, KC])
    g3, r_g3 = load_const(c, "g3", g3_d[:, :], [128, KC])
    if moe:
        rt = c.sb("rt", [128, KC, 8], F32)
        r_rt = Reg()
        c.dma("sp", rt[:], rt_d.rearrange("p (k n) -> p k n", n=8), r_rt, writes=[r_rt])

    S0b = c.sb("S0b", [128, 12, 257], BF16)
    r_S0b = Reg()
    h0 = c.sb("h0", [128, 8], F32)
    r_h0 = Reg()
    c.op("pool", lambda g: g.memset(h0[:], 0.0), writes=[r_h0])
    scope = ExitStack()
    S0 = scope.enter_context(nc.sbuf_tensor("sb_S0", [128, 12, 257], F32))
    stg = scope.enter_context(nc.sbuf_tensor("sb_stg", [128, 12, 257], F32))
    pdt = scope.enter_context(nc.sbuf_tensor("sb_pdt", [128, 12], F32))
    prg = scope.enter_context(nc.sbuf_tensor("sb_prg", [128, 16], F32))
    r_S0 = Reg()
    c.op("pool", lambda g: g.memset(S0[:], 0.0), writes=[r_S0])
    r_stg = Reg()
    r_pdt = Reg()
    r_prg = Reg()
    for j in range(7):
        c.dma("sp", stg[:], PS[j].rearrange("p (h v) -> p h v", v=257), r_stg, writes=[r_stg])
        c.dma("sp", pdt[:], PD[j], r_pdt, writes=[r_pdt])
        c.dma("sp", prg[:], PRG[j], r_prg, writes=[r_prg])
        for hh in range(12):
            c.op("dve", lambda v, hh=hh: v.scalar_tensor_tensor(out=S0[:, hh, :], in0=S0[:, hh, :], scalar=pdt[:, hh:hh + 1],
                                                               in1=stg[:, hh, :], op0=ALU.mult, op1=ALU.add),
                 reads=[r_S0, r_pdt, r_stg], writes=[r_S0])
        c.op("dve", lambda v: v.tensor_tensor(out=h0[:], in0=h0[:], in1=prg[:, 8:16], op=ALU.mult), reads=[r_h0, r_prg], writes=[r_h0])
        c.op("dve", lambda v: v.tensor_tensor(out=h0[:], in0=h0[:], in1=prg[:, 0:8], op=ALU.add), reads=[r_h0, r_prg], writes=[r_h0])
    c.op("act", lambda a: a.copy(out=S0b[:], in_=S0[:]), reads=[r_S0], writes=[r_S0b])
    for e in c.eng:
        c._waits(e, [r_S0b, r_h0], [])
    scope.close()

    XK = max(32, KC)
    xT = c.sb("xT", [128, XK, 512], BF16)
    r_xT = Reg()
    H = c.sb("H", [128, 4, D], F32)
    r_H = [Reg() for _ in range(4)]

    NSL = 2
    warena = [c.sb(f"wa{i}", [128, 12288], BF16) for i in range(NSL)]
    r_w1 = [Reg() for _ in range(NSL)]
    r_w3 = [Reg() for _ in range(NSL)]
    r_w2 = [Reg() for _ in range(NSL)]
    wc = [0]

    pmB = c.ps("pmB", [128, 512], F32)
    pc = pmB[:, 0:257]
    r_pc = Reg()
    plg = pmB[:, 264:272]
    r_plg = r_pc
    ptb = B["norm"][6]
    r_ptb = B["norm"][7]
    pa = [c.ps(f"pa{i}", [128, 512], F32) for i in range(2)]
    r_pa = [Reg(), Reg()]
    pb = [c.ps(f"pb{i}", [128, 512], F32) for i in range(2)]
    r_pb = [Reg(), Reg()]
    py = [c.ps(f"py{i}", [128, 512], F32) for i in range(2)]
    r_py = [Reg(), Reg()]
    pyc = [0]

    qgb = [c.sb(f"qgb{i}", [128, 128], BF16) for i in range(2)]
    r_qgb = [Reg(), Reg()]
    olt = [c.sb(f"olt{i}", [128, 257], F32) for i in range(2)]
    r_olt = [Reg(), Reg()]
    gtt = [c.sb(f"gtt{i}", [128, 2, 128], F32) for i in range(2)]
    r_gtt = [Reg(), Reg()]
    ot = c.sb("ot", [128, 257], F32)
    r_ot = Reg()
    oj = B["norm"][2][:, 0:256]
    r_oj = B["norm"][3]
    ob = c.sb("ob", [128, 256], F32)
    r_ob = Reg()
    sm = c.sb("sm", [128, 8], F32)
    r_smr = Reg()
    rg2 = [c.sb(f"rg2_{i}", [128, 512], F32) for i in range(2)]
    r_rg2 = [Reg(), Reg()]
    hid = [c.sb(f"hid{i}", [128, 512], BF16) for i in range(2)]
    r_hid = [Reg(), Reg()]
    sa = [c.sb(f"sa{i}", [128, 512], F32) for i in range(2)]
    r_sa = [Reg(), Reg()]
    rg1, r_rg1 = sa, r_sa
    wp_s = [c.sb(f"wp_s{i}", [128, 2, 256], BF16) for i in range(2)]
    r_wp_s = [Reg(), Reg()]
    pT_s = c.sb("pT_s", [128, 2, 512], BF16)
    r_pT_s = Reg()
    if final:
        gfs = c.sb("gfs", [128, D // 2], F32)
        r_gfs = Reg()
    comb = c.sb("comb", [128, 4, 8], F32)
    r_comb = [Reg() for _ in range(4)]
    lg = c.sb("lg", [128, 32], F32)
    r_lg = Reg()
    xt32 = [c.sb(f"xt32_{i}", [128, 4, 128], F32) for i in range(2)]
    r_xt32 = [Reg(), Reg()]

    def kc_of(hh, half):
        if hh < 4:
            return hh * 2 + half
        if hh < 8:
            return 16 + (hh - 4) * 2 + half
        return 24 + (hh - 8) * 2 + half

    for st in range(NST):
        tok0 = st * 512
        for tt in range(4):
            c.dma("sp", H[:, tt, :], hin[tok0 + tt * 128: tok0 + (tt + 1) * 128, :], r_H[tt], writes=[r_H[tt]])
        it = 0
        for hh in range(12):
            is_ml = 4 <= hh < 8
            for tt in range(4):
                i = it % 2
                it += 1
                t0 = tok0 + tt * 128
                c.dma("pool", qgb[i][:], QG[hh, :, t0:t0 + 128], r_qgb[i], writes=[r_qgb[i]])
                c.dma("sp", olt[i][:], OL[hh, t0:t0 + 128, :], r_olt[i], writes=[r_olt[i]])
                c.dma("sp", gtt[i][:], GT[hh * 2:hh * 2 + 2, :, t0:t0 + 128].rearrange("c p t -> p c t"), r_gtt[i],
                      writes=[r_gtt[i]])
                c.op("pe", lambda p, i=i, hh=hh: p.matmul(pc[:, :], lhsT=qgb[i][:], rhs=S0b[:, hh, :], start=True, stop=True),
                     reads=[r_qgb[i], r_S0b], writes=[r_pc])
                c.op("dve", lambda v, i=i: v.tensor_tensor(out=ot[:], in0=pc[:, :], in1=olt[i][:], op=ALU.add),
                     reads=[r_pc, r_olt[i]], writes=[r_ot])
                if is_ml:
                    c.op("act", lambda a: a.activation(out=sm[:, 6:7], in_=ot[:, 256:257], func=AF.Abs), reads=[r_ot], writes=[r_smr])
                    c.op("dve", lambda v: v.tensor_scalar(out=sm[:, 0:1], in0=sm[:, 6:7], scalar1=1.0, scalar2=None,
                                                          op0=ALU.max), reads=[r_smr], writes=[r_smr])
                    c.op("dve", lambda v: v.reciprocal(out=sm[:, 1:2], in_=sm[:, 0:1]), reads=[r_smr], writes=[r_smr])
                    c.op("act", lambda a: a.activation(out=oj, in_=ot[:, 0:256], func=AF.Square, scale=sm[:, 1:2],
                                                       accum_out=sm[:, 2:3]), reads=[r_ot, r_smr], writes=[r_oj, r_smr])
                else:
                    c.op("act", lambda a: a.activation(out=oj, in_=ot[:, 0:256], func=AF.Square, accum_out=sm[:, 2:3]),
                         reads=[r_ot], writes=[r_oj, r_smr])
                c.op("dve", lambda v: v.tensor_scalar(out=sm[:, 3:4], in0=sm[:, 2:3], scalar1=1.0 / 256, scalar2=EPS,
                                                      op0=ALU.mult, op1=ALU.add), reads=[r_smr], writes=[r_smr])
                c.op("act", lambda a: a.sqrt(out=sm[:, 5:6], in_=sm[:, 3:4]), reads=[r_smr], writes=[r_smr])
                c.op("dve", lambda v: v.reciprocal(out=sm[:, 4:5], in_=sm[:, 5:6]), reads=[r_smr], writes=[r_smr])
                if is_ml:
                    c.op("dve", lambda v: v.tensor_tensor(out=sm[:, 4:5], in0=sm[:, 4:5], in1=sm[:, 1:2], op=ALU.mult),
                         reads=[r_smr], writes=[r_smr])
                c.op("dve", lambda v: v.tensor_scalar(out=ob[:], in0=ot[:, 0:256], scalar1=sm[:, 4:5], scalar2=None, op0=ALU.mult),
                     reads=[r_ot, r_smr], writes=[r_ob])
                for half in range(2):
                    c.op("pe", lambda p, half=half: p.transpose(ptb[:, half, :], ob[:, half * 128:(half + 1) * 128], B["norm"][8][:]),
                         reads=[r_ob, B["norm"][9]], writes=[r_ptb], sig=(half == 1))
                for half in range(2):
                    gi = hh * 2 + half
                    c.op("dve", lambda v, half=half, gi=gi, i=i, tt=tt, hh=hh: v.scalar_tensor_tensor(
                        out=xT[:, kc_of(hh, half), tt * 128:(tt + 1) * 128], in0=ptb[:, half, :], scalar=gmix[:, gi:gi + 1],
                        in1=gtt[i][:, half, :], op0=ALU.mult, op1=ALU.mult),
                        reads=[r_ptb, r_gmix, r_gtt[i]], writes=[r_xT])
        for n in range(8):
            i = n % 2
            c.dma("sp", rg1[i][:], RG1[n, :, tok0:tok0 + 512], r_rg1[i], writes=[r_rg1[i]])
            c.dma("sp", rg2[i][:], RG2[n, :, tok0:tok0 + 512], r_rg2[i], writes=[r_rg2[i]])
            c.op("dve", lambda v, i=i, n=n: v.scalar_tensor_tensor(out=xT[:, 8 + n, :], in0=rg2[i][:], scalar=h0[:, n:n + 1],
                                                                  in1=rg1[i][:], op0=ALU.mult, op1=ALU.add),
                 reads=[r_rg1[i], r_rg2[i], r_h0], writes=[r_xT])
        wo_slots = {}

        def load_wo(ct):
            s = wc[0] % NSL
            wc[0] += 1
            wt = warena[s][:, 0:32 * 256].rearrange("p (k n) -> p k n", n=256)
            c.dma("pool", wt, wo_d[ct].rearrange("p (k n) -> p k n", n=256), r_w1[s], writes=[r_w1[s], r_w3[s]])
            wo_slots[ct] = (s, wt)

        load_wo(0)
        for ct in range(NCT):
            if ct + 1 < NCT:
                load_wo(ct + 1)
            s, wt = wo_slots[ct]
            for tt in range(4):
                i = pyc[0] % 2
                pyc[0] += 1
                for kc in range(32):
                    c.op("pe", lambda p, kc=kc, i=i, tt=tt, wt=wt: p.matmul(py[i][:, 0:256], lhsT=xT[:, kc, tt * 128:(tt + 1) * 128],
                                                                          rhs=wt[:, kc, :], start=(kc == 0), stop=(kc == 31)),
                         reads=[r_xT, r_w1[s]], writes=[r_py[i]], sig=(kc == 31))
                c.op("dve", lambda v, i=i, tt=tt, ct=ct: v.tensor_tensor(out=H[:, tt, ct * 256:(ct + 1) * 256], in0=py[i][:, 0:256],
                                                                         in1=H[:, tt, ct * 256:(ct + 1) * 256], op=ALU.add),
                     reads=[r_py[i], r_H[tt]], writes=[r_H[tt]])
        for tt in range(4):
            if not moe:
                emit_norm_tile(c, cfg, B, None, tt * 128, 128, xT, r_xT, g2, r_g2, keep=(H[:, tt, :], r_H[tt]))
            else:
                emit_norm_tile(c, cfg, B, None, tt * 128, 128, xT, r_xT, g2, r_g2, keep=(H[:, tt, :], r_H[tt]),
                               router=(xt32, r_xt32, rt, r_rt, plg, r_plg))
                c.op("dve", lambda v: v.tensor_copy(out=lg[:, 0:8], in_=plg[:, :]), reads=[r_plg], writes=[r_lg])
                c.op("dve", lambda v: v.reduce_max(out=lg[:, 24:25], in_=lg[:, 0:8], axis=mybir.AxisListType.X), reads=[r_lg], writes=[r_lg])
                c.op("dve", lambda v: v.tensor_scalar(out=lg[:, 8:16], in0=lg[:, 0:8], scalar1=lg[:, 24:25], scalar2=None, op0=ALU.is_equal),
                     reads=[r_lg], writes=[r_lg])
                c.op("dve", lambda v: v.scalar_tensor_tensor(out=lg[:, 16:24], in0=lg[:, 8:16], scalar=-1e30, in1=lg[:, 0:8],
                                                             op0=ALU.mult, op1=ALU.add), reads=[r_lg], writes=[r_lg])
                c.op("dve", lambda v: v.reduce_max(out=lg[:, 25:26], in_=lg[:, 16:24], axis=mybir.AxisListType.X), reads=[r_lg], writes=[r_lg])
                c.op("dve", lambda v: v.tensor_scalar(out=lg[:, 16:24], in0=lg[:, 16:24], scalar1=lg[:, 25:26], scalar2=None, op0=ALU.is_equal),
                     reads=[r_lg], writes=[r_lg])
                c.op("dve", lambda v: v.tensor_tensor(out=lg[:, 26:27], in0=lg[:, 25:26], in1=lg[:, 24:25], op=ALU.subtract),
                     reads=[r_lg], writes=[r_lg])
                c.op("act", lambda a: a.activation(out=lg[:, 27:28], in_=lg[:, 26:27], func=AF.Exp), reads=[r_lg], writes=[r_lg])
                c.op("dve", lambda v: v.tensor_scalar(out=lg[:, 28:29], in0=lg[:, 27:28], scalar1=1.0, scalar2=None, op0=ALU.add),
                     reads=[r_lg], writes=[r_lg])
                c.op("dve", lambda v: v.reciprocal(out=lg[:, 29:30], in_=lg[:, 28:29]), reads=[r_lg], writes=[r_lg])
                c.op("dve", lambda v: v.tensor_tensor(out=lg[:, 30:31], in0=lg[:, 27:28], in1=lg[:, 29:30], op=ALU.mult),
                     reads=[r_lg], writes=[r_lg])
                c.op("dve", lambda v: v.tensor_scalar(out=lg[:, 8:16], in0=lg[:, 8:16], scalar1=lg[:, 29:30], scalar2=None, op0=ALU.mult),
                     reads=[r_lg], writes=[r_lg])
                c.op("dve", lambda v, tt=tt: v.scalar_tensor_tensor(out=comb[:, tt, :], in0=lg[:, 16:24], scalar=lg[:, 30:31],
                                                                   in1=lg[:, 8:16], op0=ALU.mult, op1=ALU.add),
                     reads=[r_lg], writes=[r_comb[tt]])
        chunks = [(e, jc) for e in range(NE if moe else 1) for jc in range(NHC)]
        NCHK = len(chunks)
        slots = {}
        NYS = 4 * (D // 512)
        APER = -(-(2 * KC) // NYS)

        wc_base = wc[0]
        wc[0] += NCHK

        def _slot(ci):
            e, jc = chunks[ci]
            cidx = e * NHC + jc
            s = (wc_base + ci) % NSL
            w1t = warena[s][:, 0:KC * 128].rearrange("p (k n) -> p k n", n=128)
            w3t = warena[s][:, 4096:4096 + KC * 128].rearrange("p (k n) -> p k n", n=128)
            w2t = warena[s][:, 8192:8192 + D]
            slots[ci] = (s, w1t, w3t, w2t)
            return cidx, s, w1t, w3t, w2t

        def load_a(ci):
            cidx, s, w1t, w3t, w2t = _slot(ci)
            c.dma("pool", w1t, w1_d[cidx].rearrange("p (k n) -> p k n", n=128), r_w1[s], writes=[r_w1[s]])
            c.dma("pool", w3t, w3_d[cidx].rearrange("p (k n) -> p k n", n=128), r_w3[s], writes=[r_w3[s]])

        def load_y(ci):
            cidx, s, w1t, w3t, w2t = _slot(ci)
            c.dma("pool", w2t, w2_d[cidx * 128:(cidx + 1) * 128, :], r_w2[s], writes=[r_w2[s]])

        def a_mm(ci, m):
            s, w1t, w3t, w2t = slots[ci]
            i = ci % 2
            if m < KC:
                c.op("pe", lambda p, kc=m, i=i, w1t=w1t: p.matmul(pa[i][:, :], lhsT=w1t[:, kc, :], rhs=xT[:, kc, :],
                                                                 start=(kc == 0), stop=(kc == KC - 1)),
                     reads=[r_w1[s], r_xT], writes=[r_pa[i]], sig=(m == KC - 1))
            else:
                kc = m - KC
                c.op("pe", lambda p, kc=kc, i=i, w3t=w3t: p.matmul(pb[i][:, :], lhsT=w3t[:, kc, :], rhs=xT[:, kc, :],
                                                                  start=(kc == 0), stop=(kc == KC - 1)),
                     reads=[r_w3[s], r_xT], writes=[r_pb[i]], sig=(kc == KC - 1))

        def a_epi(ci):
            i = ci % 2
            c.op("act", lambda a, i=i: a.activation(out=sa[i][:], in_=pa[i][:, :], func=AF.Silu), reads=[r_pa[i]], writes=[r_sa[i]])
            c.op("dve", lambda v, i=i: v.tensor_tensor(out=hid[i][:], in0=pb[i][:, :], in1=sa[i][:], op=ALU.mult),
                 reads=[r_pb[i], r_sa[i]], writes=[r_hid[i]])

        def y_step(ci, k):
            s, w1t, w3t, w2t = slots[ci]
            e = chunks[ci][0]
            i = ci % 2
            tt = k // (D // 512)
            n0 = (k % (D // 512)) * 512
            yi = pyc[0] % 2
            pyc[0] += 1
            c.op("pe", lambda p, yi=yi, i=i, tt=tt, n0=n0, w2t=w2t: p.matmul(
                py[yi][:, :], lhsT=hid[i][:, tt * 128:(tt + 1) * 128], rhs=w2t[:, n0:n0 + 512], start=True, stop=True),
                reads=[r_hid[i], r_w2[s]], writes=[r_py[yi]])
            if moe:
                c.op("dve", lambda v, yi=yi, tt=tt, n0=n0, e=e: v.scalar_tensor_tensor(
                    out=H[:, tt, n0:n0 + 512], in0=py[yi][:, :], scalar=comb[:, tt, e:e + 1], in1=H[:, tt, n0:n0 + 512],
                    op0=ALU.mult, op1=ALU.add), reads=[r_py[yi], r_comb[tt], r_H[tt]], writes=[r_H[tt]])
            else:
                c.op("dve", lambda v, yi=yi, tt=tt, n0=n0: v.tensor_tensor(
                    out=H[:, tt, n0:n0 + 512], in0=py[yi][:, :], in1=H[:, tt, n0:n0 + 512], op=ALU.add),
                    reads=[r_py[yi], r_H[tt]], writes=[r_H[tt]])

        load_a(0)
        load_y(0)
        if NCHK > 1:
            load_a(1)
        for m in range(2 * KC):
            a_mm(0, m)
        a_epi(0)
        if NCHK > 1:
            load_y(1)
        for ci in range(1, NCHK):
            if ci + 1 < NCHK:
                load_a(ci + 1)
            m = 0
            for k in range(NYS):
                for _ in range(APER):
                    if m < 2 * KC:
                        a_mm(ci, m)
                        m += 1
                y_step(ci - 1, k)
            while m < 2 * KC:
                a_mm(ci, m)
                m += 1
            a_epi(ci)
            if ci + 1 < NCHK:
                load_y(ci + 1)
        for k in range(NYS):
            y_step(NCHK - 1, k)
        for tt in range(4):
            emit_norm_tile(c, cfg, B, None, tt * 128, 128, xT, r_xT, g3, r_g3, keep=(H[:, tt, :], r_H[tt]))
        c.dma("pool", pT_s[:], pT_d.rearrange("p (k n) -> p k n", n=T)[:, :, tok0:tok0 + 512], r_pT_s, writes=[r_pT_s])
        wg_slots = {}

        def load_wg(ct):
            s = wc[0] % NSL
            wc[0] += 1
            wt = warena[s][:, 0:KC * 256].rearrange("p (k n) -> p k n", n=256)
            c.dma("pool", wt, wg_d[ct].rearrange("p (k n) -> p k n", n=256), r_w1[s], writes=[r_w1[s], r_w3[s]])
            wi = ct % 2
            c.dma("pool", wp_s[wi][:], wp_d.rearrange("p (k n) -> p k n", n=D)[:, :, ct * 256:(ct + 1) * 256], r_wp_s[wi],
                  writes=[r_wp_s[wi]])
            wg_slots[ct] = (s, wt)

        load_wg(0)
        for ct in range(NCT):
            if ct + 1 < NCT:
                load_wg(ct + 1)
            s, wt = wg_slots[ct]
            for tt in range(4):
                i = (ct * 4 + tt) % 2
                for kc in range(KC):
                    c.op("pe", lambda p, kc=kc, i=i, tt=tt, wt=wt: p.matmul(pa[i][:, 0:256], lhsT=xT[:, kc, tt * 128:(tt + 1) * 128],
                                                                          rhs=wt[:, kc, :], start=(kc == 0), stop=(kc == KC - 1)),
                         reads=[r_xT, r_w1[s]], writes=[r_pa[i]], sig=(kc == KC - 1))
                for k2 in range(2):
                    c.op("pe", lambda p, k2=k2, i=i, tt=tt, ct=ct: p.matmul(
                        pb[i][:, 0:256], lhsT=pT_s[:, k2, tt * 128:(tt + 1) * 128],
                        rhs=wp_s[ct % 2][:, k2, :], start=(k2 == 0), stop=(k2 == 1)),
                        reads=[r_pT_s, r_wp_s[ct % 2]], writes=[r_pb[i]], sig=(k2 == 1))
                c.op("act", lambda a, i=i: a.activation(out=sa[i][:, 0:256], in_=pa[i][:, 0:256], func=AF.Sigmoid),
                     reads=[r_pa[i]], writes=[r_sa[i]])
                c.op("dve", lambda v, i=i: v.tensor_tensor(out=sa[i][:, 0:256], in0=pb[i][:, 0:256], in1=sa[i][:, 0:256], op=ALU.mult),
                     reads=[r_pb[i], r_sa[i]], writes=[r_sa[i]])
                c.op("dve", lambda v, i=i, tt=tt, ct=ct: v.tensor_tensor(out=H[:, tt, ct * 256:(ct + 1) * 256], in0=sa[i][:, 0:256],
                                                                         in1=H[:, tt, ct * 256:(ct + 1) * 256], op=ALU.add),
                     reads=[r_sa[i], r_H[tt]], writes=[r_H[tt]])
        for tt in range(4):
            t0 = tok0 + tt * 128
            if not final:
                c.dma("sp", hout[t0:t0 + 128, :], H[:, tt, :], r_H[tt], reads=[r_H[tt]])
            else:
                hs, r_hs, hn, r_hn, ss, r_ss, ptr, r_ptr, ident, r_id = B["norm"]
                sap = H[:, tt, :]
                DH = D // 2
                for hf in range(2):
                    c.op("act", lambda a, sap=sap, hf=hf: a.activation(out=hn[:], in_=sap[:, hf * DH:(hf + 1) * DH], func=AF.Square,
                                                                       accum_out=ss[:, 4 + hf:5 + hf]),
                         reads=[r_H[tt]], writes=[r_hn, r_ss])
                c.op("dve", lambda v: v.tensor_tensor(out=ss[:, 0:1], in0=ss[:, 4:5], in1=ss[:, 5:6], op=ALU.add),
                     reads=[r_ss], writes=[r_ss])
                c.op("dve", lambda v: v.tensor_scalar(out=ss[:, 1:2], in0=ss[:, 0:1], scalar1=1.0 / D, scalar2=EPS,
                                                      op0=ALU.mult, op1=ALU.add), reads=[r_ss], writes=[r_ss])
                c.op("act", lambda a: a.sqrt(out=ss[:, 3:4], in_=ss[:, 1:2]), reads=[r_ss], writes=[r_ss])
                c.op("dve", lambda v: v.reciprocal(out=ss[:, 2:3], in_=ss[:, 3:4]), reads=[r_ss], writes=[r_ss])
                for hf in range(2):
                    c.dma("sp", gfs[:], gf_d[:, hf * DH:(hf + 1) * DH], r_gfs, writes=[r_gfs])
                    c.op("dve", lambda v, sap=sap, hf=hf: v.scalar_tensor_tensor(out=hn[:], in0=sap[:, hf * DH:(hf + 1) * DH],
                                                                                scalar=ss[:, 2:3], in1=gfs[:],
                                                                                op0=ALU.mult, op1=ALU.mult),
                         reads=[r_H[tt], r_ss, r_gfs], writes=[r_hn])
                    c.dma("sp", hout[t0:t0 + 128, hf * DH:(hf + 1) * DH], hn[:], r_hn, reads=[r_hn])
    c.finish()
    return nc


def _pk(v, KC):
    return np.ascontiguousarray(np.asarray(v, np.float32).reshape(KC, 128).T)


def _chunkw(W, ncols):
    K, N = W.shape
    return np.ascontiguousarray(W.reshape(K // 128, 128, N // ncols, ncols).transpose(2, 1, 0, 3)).reshape(
        N // ncols, 128, (K // 128) * ncols)


_FC = fchunk_cols()


def _prep_M(cfg, l, inp):
    KC = cfg["KC"]
    w_in = np.asarray(inp["w_in"][l], np.float32)
    D = w_in.shape[0]
    wf = np.zeros((NFCH, 128, KC, 128), np.float32)
    wr = w_in.reshape(KC, 128, -1)
    for ci, cols in enumerate(_FC):
        ca = np.array(cols)
        ok = ca >= 0
        blk = np.zeros((KC, 128, 128), np.float32)
        blk[:, :, ok] = wr[:, :, ca[ok]]
        wf[ci] = blk.transpose(1, 0, 2)
    wf = wf.reshape(NFCH, 128, KC * 128)
    wv = np.zeros((12, 128, KC, 256), np.float32)
    for hh in range(12):
        wv[hh] = wr[:, :, V_OFF[hh]:V_OFF[hh] + 256].transpose(1, 0, 2)
    wv = wv.reshape(12, 128, KC * 256)
    sp = np.zeros((128, 96), np.float32)
    sp[:, 0:4] = np.asarray(inp["gla_b_up"][l]).reshape(4, 128).T
    sp[:, 4:8] = np.asarray(inp["ml_b_i"][l])[None, :]
    sp[:, 8:12] = np.asarray(inp["ml_b_f"][l])[None, :]
    sp[:, 12:16] = np.asarray(inp["hg_lb_logits"][0]).reshape(4, 128).T
    sp[:, 16:20] = np.asarray(inp["hg_lb_logits"][min(l, 1)]).reshape(4, 128).T if l > 0 else 0.0
    sp[:, 20:28] = np.asarray(inp["rg_conv_b"][l]).reshape(8, 128).T
    cw = np.asarray(inp["rg_conv_w"][l])
    sp[:, 28:60] = cw.reshape(4, 8, 128).transpose(2, 1, 0).reshape(128, 32)
    sp[:, 60:68] = np.asarray(inp["rg_b_a"][l]).reshape(8, 128).T
    sp[:, 68:76] = np.asarray(inp["rg_b_x"][l]).reshape(8, 128).T
    sp[:, 76:84] = np.asarray(inp["rg_lambda"][l]).reshape(8, 128).T
    wax = np.concatenate([np.asarray(inp["rg_w_a"][l]), np.asarray(inp["rg_w_x"][l])], axis=0)
    wax = np.ascontiguousarray(wax.transpose(1, 0, 2))
    return dict(gain=_pk(inp["attn_norm"][l], KC), wf=wf, wv=wv, wup=np.asarray(inp["gla_w_up"][l], np.float32),
                smallp=sp, wax=wax)


def _prep_R(cfg, l, inp, moe, final):
    KC, D = cfg["KC"], cfg["D"]
    j = l // 2
    d = {}
    gm = np.zeros((128, 24), np.float32)
    gl = np.asarray(inp["gla_norm"][l]).reshape(2, 128).T
    hg = np.asarray(inp["hg_norm"][l]).reshape(2, 128).T
    ml = np.asarray(inp["ml_norm"][l]).reshape(8, 128).T
    for h in range(4):
        gm[:, h * 2:h * 2 + 2] = gl
        gm[:, 8 + h * 2:8 + h * 2 + 2] = ml[:, h * 2:h * 2 + 2]
        gm[:, 16 + h * 2:16 + h * 2 + 2] = hg
    d["gmix"] = gm
    d["wo"] = _chunkw(np.asarray(inp["w_out"][l], np.float32), 256)
    d["g2"] = _pk(inp["ffn_norm"][l], KC)
    d["g3"] = _pk(inp["ple_norm"][l], KC)
    if moe:
        w1 = np.asarray(inp["moe_w1"][j], np.float32)
        w3 = np.asarray(inp["moe_w3"][j], np.float32)
        w2 = np.asarray(inp["moe_w2"][j], np.float32)
        d["w1"] = np.concatenate([_chunkw(w1[e], 128) for e in range(w1.shape[0])], axis=0)
        d["w3"] = np.concatenate([_chunkw(w3[e], 128) for e in range(w3.shape[0])], axis=0)
        d["w2"] = np.ascontiguousarray(w2.reshape(-1, D))
        r = np.asarray(inp["moe_router"][j], np.float32)
        d["router"] = np.ascontiguousarray(r.reshape(KC, 128, 8).transpose(1, 0, 2)).reshape(128, KC * 8)
    else:
        d["w1"] = _chunkw(np.asarray(inp["ffn_w1"][j], np.float32), 128)
        d["w3"] = _chunkw(np.asarray(inp["ffn_w3"][j], np.float32), 128)
        d["w2"] = np.ascontiguousarray(np.asarray(inp["ffn_w2"][j], np.float32))
    d["wg"] = _chunkw(np.asarray(inp["ple_w_gate"][l], np.float32), 256)
    wp = np.asarray(inp["ple_w_proj"][l], np.float32)
    d["wp"] = np.ascontiguousarray(wp.reshape(2, 128, D).transpose(1, 0, 2)).reshape(128, 2 * D)
    if final:
        d["gf"] = np.ascontiguousarray(np.broadcast_to(np.asarray(inp["final_norm"], np.float32)[None, :], (128, D)))
    return d


_PROG = {}


def _get_prog(kind, cfg, *args):
    key = (kind, tuple(sorted(cfg.items())), args)
    if key not in _PROG:
        _PROG[key] = build_M(cfg, *args) if kind == "M" else build_R(cfg, *args)
    return _PROG[key]


def run_model(cfg, inp):
    NCORE, T, D = cfg["NCORE"], cfg["T"], cfg["D"]
    x = np.asarray(inp["x"], np.float32).reshape(-1, D)
    p = np.asarray(inp["p"], np.float32)
    depth = p.shape[0]
    h = x
    cores = list(range(NCORE))
    for l in range(depth):
        moe = (l % 2 == 1)
        final = (l == depth - 1)
        wM = _prep_M(cfg, l, inp)
        maps = []
        for cidx in cores:
            halo = h[cidx * T - 128: cidx * T] if cidx > 0 else np.zeros((128, D), np.float32)
            m = dict(wM)
            m["hin"] = np.ascontiguousarray(np.concatenate([halo, h[cidx * T:(cidx + 1) * T]], axis=0))
            maps.append(m)
        resM = run_bass_kernel_spmd(_get_prog("M", cfg, l), maps, core_ids=cores).results
        del maps, wM
        wR = _prep_R(cfg, l, inp, moe, final)
        pT = np.ascontiguousarray(p[l].reshape(-1, 256).T)
        maps = []
        for cidx in cores:
            m = dict(wR)
            m["hin"] = np.ascontiguousarray(h[cidx * T:(cidx + 1) * T])
            for k in ("OL", "QG", "GT", "RG1", "RG2"):
                m[k] = resM[cidx][k]
            PS = np.zeros((7, 128, 12 * 257), np.float32)
            PD = np.ones((7, 128, 12), np.float32)
            PRG = np.zeros((7, 128, 16), np.float32)
            PRG[:, :, 8:16] = 1.0
            for jj in range(cidx):
                PS[jj] = resM[jj]["STo"].reshape(128, 12 * 257)
                PD[jj] = resM[jj]["SDo"]
                PRG[jj] = resM[jj]["RGS"]
            m["PS"], m["PD"], m["PRG"] = PS, PD, PRG
            pc_ = pT[:, cidx * T:(cidx + 1) * T]
            m["pT"] = np.ascontiguousarray(pc_.reshape(2, 128, T).transpose(1, 0, 2)).reshape(128, 2 * T)
            maps.append(m)
        resR = run_bass_kernel_spmd(_get_prog("R", cfg, l, moe, final), maps, core_ids=cores).results
        del maps, wR
        h = np.concatenate([resR[cidx]["hout"] for cidx in cores], axis=0)
    return h


def kernel(**inputs):
    cfg = make_cfg()
    x = inputs["x"]
    out = run_model(cfg, inputs)
    return out.reshape(x.shape).astype(np.float32, copy=False)
```
